# Optimizing a Trainium2 kernel written in Bass

```python
import math
import jax, jax.numpy as jnp
from jax import lax
import numpy as np

D_MODEL = 1024
BATCH = 16
SEQ = 2048
DEPTH = 2

MEM_LEN = 256
N_MOBA_LAYERS = (DEPTH + 1) // 2
N_SSD_LAYERS = DEPTH // 2

MOBA_HEADS = 12
MOBA_HD = 128
MOBA_WIDTH = MOBA_HEADS * MOBA_HD
MOBA_BLOCK = 256
MOBA_TOPK = 3
MOBA_QCHUNK = 128

REL_BUCKETS = 32
REL_MAX_DIST = 128

MEM_HEADS = 4
MEM_HD = 128
MEM_WIDTH = MEM_HEADS * MEM_HD

SSD_HEADS = 24
SSD_HD = 64
SSD_INNER = SSD_HEADS * SSD_HD
SSD_GROUPS = 4
SSD_STATE = 128
SSD_CONV = 4
SSD_CHUNK = 128
SSD_CONV_DIM = SSD_INNER + 2 * SSD_GROUPS * SSD_STATE

MIX_WIDTH = MOBA_WIDTH + MEM_WIDTH
IN_MOBA = 3 * MOBA_WIDTH + MEM_WIDTH
IN_SSD = SSD_INNER + SSD_CONV_DIM + SSD_HEADS + MEM_WIDTH

D_FF = 3584
N_EXPERTS = 8
TOP_K = 2
EXP_CHUNK = 256

EPS = 1e-6

kernel_name = 'hybrid_moba_ssd_moe_block'


def rms_norm(x, g):
    xf = x.astype(jnp.float32)
    y = xf * lax.rsqrt(jnp.mean(xf * xf, axis=-1, keepdims=True) + EPS)
    return (y * g.astype(jnp.float32)).astype(x.dtype)


def t5_bucket(dist):
    n = jnp.maximum(dist, 0)
    max_exact = REL_BUCKETS // 2
    large = max_exact + (jnp.log(jnp.maximum(n, 1).astype(jnp.float32) / max_exact)
                         / math.log(REL_MAX_DIST / max_exact)
                         * (REL_BUCKETS - max_exact)).astype(jnp.int32)
    large = jnp.minimum(large, REL_BUCKETS - 1)
    return jnp.where(n < max_exact, n, large)


def bucket_layout(ids, valid, n_buckets, chunk, n_buf):
    n_items = ids.shape[0]
    oh = ((ids[:, None] == jnp.arange(n_buckets)[None, :]) & valid[:, None]).astype(jnp.int32)
    counts = oh.sum(axis=0)
    rank = jnp.take_along_axis(jnp.cumsum(oh, axis=0) - oh, ids[:, None], axis=1)[:, 0]
    padded = (counts + chunk - 1) // chunk * chunk
    ends = jnp.cumsum(padded)
    starts = ends - padded
    dest = jnp.where(valid, starts[ids] + rank, n_buf)
    src = jnp.full((n_buf,), n_items, jnp.int32).at[dest].set(jnp.arange(n_items, dtype=jnp.int32), mode='drop')
    chunk_start = jnp.arange(n_buf // chunk) * chunk
    chunk_ids = jnp.minimum(jnp.sum(chunk_start[:, None] >= ends[None, :], axis=1), n_buckets - 1)
    return dest, src, chunk_ids


def moba_head(q, k, v, bias_h):
    s_pad = q.shape[0]
    nb = s_pad // MOBA_BLOCK
    n_sel = min(MOBA_TOPK, nb - 1)
    scale = MOBA_HD ** -0.5
    qb = q.reshape(nb, MOBA_BLOCK, MOBA_HD)
    kb = k.reshape(nb, MOBA_BLOCK, MOBA_HD)
    vb = v.reshape(nb, MOBA_BLOCK, MOBA_HD)
    loc = jnp.arange(MOBA_BLOCK)
    dist_self = loc[:, None] - loc[None, :]
    s_self = jnp.einsum('nqd,nkd->nqk', qb, kb).astype(jnp.float32) * scale + bias_h[t5_bucket(dist_self)]
    s_self = jnp.where(dist_self >= 0, s_self, -jnp.inf)
    lse_self = jax.nn.logsumexp(s_self, axis=-1)
    o_self = jnp.einsum('nqk,nkd->nqd', jnp.exp(s_self - lse_self[..., None]).astype(v.dtype), vb)
    o_self = o_self.reshape(s_pad, MOBA_HD)
    if n_sel == 0:
        return o_self
    lse_self = lse_self.reshape(s_pad)
    pos = jnp.arange(s_pad)
    k_mean = kb.mean(axis=1)
    gate = (q @ k_mean.T).astype(jnp.float32)
    past = jnp.arange(nb)[None, :] < (pos // MOBA_BLOCK)[:, None]
    gate = jnp.where(past, gate, -jnp.inf)
    top_s, top_i = lax.top_k(gate, n_sel)
    valid = jnp.isfinite(top_s).reshape(-1)
    ids = top_i.reshape(-1).astype(jnp.int32)
    pair_q = jnp.repeat(pos, n_sel).astype(jnp.int32)
    n_buf = s_pad * n_sel + nb * MOBA_QCHUNK
    n_ch = n_buf // MOBA_QCHUNK
    dest, src, cid = bucket_layout(ids, valid, nb, MOBA_QCHUNK, n_buf)
    buf_q = jnp.concatenate([pair_q, jnp.zeros((1,), jnp.int32)])[src]
    qg = q[buf_q].reshape(n_ch, MOBA_QCHUNK, MOBA_HD)
    kg = kb[cid]
    vg = vb[cid]
    kpos = cid[:, None] * MOBA_BLOCK + loc[None, :]
    dist = buf_q.reshape(n_ch, MOBA_QCHUNK)[:, :, None] - kpos[:, None, :]
    s_r = jnp.einsum('cqd,ckd->cqk', qg, kg).astype(jnp.float32) * scale + bias_h[t5_bucket(dist)]
    lse_r = jax.nn.logsumexp(s_r, axis=-1)
    o_r = jnp.einsum('cqk,ckd->cqd', jnp.exp(s_r - lse_r[..., None]).astype(v.dtype), vg)
    o_r = o_r.reshape(n_buf, MOBA_HD)
    take = jnp.minimum(dest, n_buf - 1)
    o_pair = o_r[take].reshape(s_pad, n_sel, MOBA_HD)
    lse_pair = jnp.where(valid, lse_r.reshape(n_buf)[take], -jnp.inf).reshape(s_pad, n_sel)
    w = jax.nn.softmax(jnp.concatenate([lse_self[:, None], lse_pair], axis=1), axis=-1)
    o_all = jnp.concatenate([o_self[:, None], o_pair], axis=1)
    return jnp.einsum('sj,sjd->sd', w.astype(o_all.dtype), o_all)


def moba_attention(qkv, rel_bias):
    bsz, s, _ = qkv.shape
    s_pad = -(-s // MOBA_BLOCK) * MOBA_BLOCK
    q, k, v = jnp.split(qkv, 3, axis=-1)

    def to_heads(t):
        t = t.reshape(bsz, s, MOBA_HEADS, MOBA_HD).transpose(0, 2, 1, 3)
        return jnp.pad(t, ((0, 0), (0, 0), (0, s_pad - s), (0, 0)))

    q, k, v = to_heads(q), to_heads(k), to_heads(v)
    bias_heads = rel_bias.T
    per_head = jax.vmap(moba_head, in_axes=(0, 0, 0, 0))

    def per_batch(args):
        qb, kb, vb = args
        return per_head(qb, kb, vb, bias_heads)

    o = lax.map(per_batch, (q, k, v))
    return o[:, :, :s].transpose(0, 2, 1, 3).reshape(bsz, s, MOBA_WIDTH)


def memory_cross_attention(q, mem_n, w_kv):
    bsz, s, _ = q.shape
    kv = mem_n @ w_kv
    k, v = jnp.split(kv, 2, axis=-1)
    q = q.reshape(bsz, s, MEM_HEADS, MEM_HD)
    k = k.reshape(bsz, -1, MEM_HEADS, MEM_HD)
    v = v.reshape(bsz, -1, MEM_HEADS, MEM_HD)
    sc = jnp.einsum('bshd,bmhd->bhsm', q, k).astype(jnp.float32) * (MEM_HD ** -0.5)
    p = jax.nn.softmax(sc, axis=-1).astype(v.dtype)
    return jnp.einsum('bhsm,bmhd->bshd', p, v).reshape(bsz, s, MEM_WIDTH)


def causal_dwconv(x, w, b):
    ksz = w.shape[0]
    y = lax.conv_general_dilated(x, w[:, None, :].astype(x.dtype), window_strides=(1,),
                                 padding=[(ksz - 1, 0)], dimension_numbers=('NWC', 'WIO', 'NWC'),
                                 feature_group_count=x.shape[-1])
    return y + b.astype(x.dtype)


def ssd_scan(x, dt, a, b_in, c_in):
    bsz, s = x.shape[0], x.shape[1]
    s_pad = -(-s // SSD_CHUNK) * SSD_CHUNK

    def pad(t):
        return jnp.pad(t, ((0, 0), (0, s_pad - s)) + ((0, 0),) * (t.ndim - 2))

    x, dt, b_in, c_in = pad(x), pad(dt), pad(b_in), pad(c_in)
    nc = s_pad // SSD_CHUNK
    e = SSD_HEADS // SSD_GROUPS
    xdt = (x * dt[..., None]).reshape(bsz, nc, SSD_CHUNK, SSD_GROUPS, e, SSD_HD)
    la = (dt * a).reshape(bsz, nc, SSD_CHUNK, SSD_GROUPS, e)
    la_cs = jnp.cumsum(la, axis=2)
    bc = b_in.reshape(bsz, nc, SSD_CHUNK, SSD_GROUPS, SSD_STATE)
    cc = c_in.reshape(bsz, nc, SSD_CHUNK, SSD_GROUPS, SSD_STATE)
    causal = (jnp.arange(SSD_CHUNK)[:, None] >= jnp.arange(SSD_CHUNK)[None, :])[None, None, :, :, None, None]
    decay = jnp.exp(jnp.where(causal, la_cs[:, :, :, None] - la_cs[:, :, None], -jnp.inf))
    scores = jnp.einsum('bclgn,bcsgn->bclsg', cc, bc)[..., None] * decay
    y_diag = jnp.einsum('bclsge,bcsgep->bclgep', scores, xdt)
    decay_to_end = jnp.exp(la_cs[:, :, -1:] - la_cs)
    states = jnp.einsum('bclgn,bclge,bclgep->bcgepn', bc, decay_to_end, xdt)
    chunk_decay = jnp.exp(la_cs[:, :, -1])

    def step(h, inp):
        st, dec = inp
        return dec[..., None, None] * h + st, h

    h0 = jnp.zeros((bsz, SSD_GROUPS, e, SSD_HD, SSD_STATE), jnp.float32)
    _, h_prev = lax.scan(step, h0, (states.transpose(1, 0, 2, 3, 4, 5), chunk_decay.transpose(1, 0, 2, 3)))
    h_prev = h_prev.transpose(1, 0, 2, 3, 4, 5)
    y_off = jnp.einsum('bclgn,bcgepn,bclge->bclgep', cc, h_prev, jnp.exp(la_cs))
    y = (y_diag + y_off).reshape(bsz, s_pad, SSD_HEADS, SSD_HD)
    return y[:, :s]


def ssd_mixer(zxbcdt, conv_w, conv_b, dt_bias, a_log, d_skip, g_out):
    bsz, s, _ = zxbcdt.shape
    z = zxbcdt[..., :SSD_INNER]
    xbc = zxbcdt[..., SSD_INNER:SSD_INNER + SSD_CONV_DIM]
    dt_raw = zxbcdt[..., SSD_INNER + SSD_CONV_DIM:]
    xbc = jax.nn.silu(causal_dwconv(xbc, conv_w, conv_b)).astype(jnp.float32)
    xs = xbc[..., :SSD_INNER]
    b_in = xbc[..., SSD_INNER:SSD_INNER + SSD_GROUPS * SSD_STATE].reshape(bsz, s, SSD_GROUPS, SSD_STATE)
    c_in = xbc[..., SSD_INNER + SSD_GROUPS * SSD_STATE:].reshape(bsz, s, SSD_GROUPS, SSD_STATE)
    dt = jax.nn.softplus(dt_raw.astype(jnp.float32) + dt_bias.astype(jnp.float32))
    a = -jnp.exp(a_log.astype(jnp.float32))
    xh = xs.reshape(bsz, s, SSD_HEADS, SSD_HD)
    y = ssd_scan(xh, dt, a, b_in, c_in) + xh * d_skip.astype(jnp.float32)[:, None]
    u = (y.reshape(bsz, s, SSD_INNER) * jax.nn.silu(z.astype(jnp.float32))).reshape(bsz, s, SSD_GROUPS, -1)
    u = u * lax.rsqrt(jnp.mean(u * u, axis=-1, keepdims=True) + EPS)
    return (u.reshape(bsz, s, SSD_INNER) * g_out.astype(jnp.float32)).astype(zxbcdt.dtype)


def swiglu(h, w_gate, w_up, w_down):
    return (jax.nn.silu(h @ w_gate) * (h @ w_up)) @ w_down


def moe_swiglu(h, w_router, b_router, w_gate, w_up, w_down):
    bsz, s, d = h.shape
    n_tok = bsz * s
    xf = h.reshape(n_tok, d)
    logits = (xf @ w_router).astype(jnp.float32) + b_router.astype(jnp.float32)
    top_l, top_e = lax.top_k(logits, TOP_K)
    gates = jax.nn.softmax(top_l, axis=-1)
    ids = top_e.reshape(-1).astype(jnp.int32)
    tok = jnp.repeat(jnp.arange(n_tok, dtype=jnp.int32), TOP_K)
    n_buf = n_tok * TOP_K + N_EXPERTS * EXP_CHUNK
    dest, src, cid = bucket_layout(ids, jnp.ones_like(ids, dtype=bool), N_EXPERTS, EXP_CHUNK, n_buf)
    buf_tok = jnp.concatenate([tok, jnp.zeros((1,), jnp.int32)])[src]
    xb = xf[buf_tok].reshape(n_buf // EXP_CHUNK, EXP_CHUNK, d)

    def expert_chunk(args):
        xc, e = args
        return (jax.nn.silu(xc @ w_gate[e]) * (xc @ w_up[e])) @ w_down[e]

    yb = lax.map(expert_chunk, (xb, cid)).reshape(n_buf, d)
    y_pair = yb[dest].reshape(n_tok, TOP_K, d)
    y = jnp.einsum('tk,tkd->td', gates.astype(y_pair.dtype), y_pair)
    return y.reshape(bsz, s, d)


def setup_inputs(seed: int = 0) -> dict:
    key = jax.random.key(seed)
    ks = iter(jax.random.split(key, 32))
    f32 = jnp.float32

    def nrm(shape, scale):
        return jax.random.normal(next(ks), shape, f32) * scale

    def gain(shape):
        return jnp.ones(shape, f32) + nrm(shape, 0.02)

    dt0 = jnp.exp(jax.random.uniform(next(ks), (N_SSD_LAYERS, SSD_HEADS), f32)
                  * (math.log(0.1) - math.log(0.001)) + math.log(0.001))
    return {
        'x': nrm((BATCH, SEQ, D_MODEL), 1.0),
        'mem': nrm((BATCH, MEM_LEN, D_MODEL), 1.0),
        'g_mix': gain((DEPTH, D_MODEL)),
        'g_mem': gain((DEPTH, D_MODEL)),
        'w_in_moba': nrm((N_MOBA_LAYERS, D_MODEL, IN_MOBA), D_MODEL ** -0.5),
        'w_in_ssd': nrm((N_SSD_LAYERS, D_MODEL, IN_SSD), D_MODEL ** -0.5),
        'w_mem_kv': nrm((DEPTH, D_MODEL, 2 * MEM_WIDTH), D_MODEL ** -0.5),
        'w_out': nrm((DEPTH, MIX_WIDTH, D_MODEL), MIX_WIDTH ** -0.5),
        'rel_bias': nrm((REL_BUCKETS, MOBA_HEADS), 0.2),
        'conv_w': nrm((N_SSD_LAYERS, SSD_CONV, SSD_CONV_DIM), SSD_CONV ** -0.5),
        'conv_b': nrm((N_SSD_LAYERS, SSD_CONV_DIM), 0.02),
        'dt_bias': dt0 + jnp.log(-jnp.expm1(-dt0)),
        'a_log': jnp.log(jax.random.uniform(next(ks), (N_SSD_LAYERS, SSD_HEADS), f32, 1.0, 16.0)),
        'd_skip': jnp.ones((N_SSD_LAYERS, SSD_HEADS), f32) + nrm((N_SSD_LAYERS, SSD_HEADS), 0.1),
        'g_ssd_out': gain((N_SSD_LAYERS, SSD_INNER)),
        'g_ffn': gain((DEPTH, D_MODEL)),
        'w_ffn_gate': nrm((N_MOBA_LAYERS, D_MODEL, D_FF), D_MODEL ** -0.5),
        'w_ffn_up': nrm((N_MOBA_LAYERS, D_MODEL, D_FF), D_MODEL ** -0.5),
        'w_ffn_down': nrm((N_MOBA_LAYERS, D_FF, D_MODEL), D_FF ** -0.5),
        'w_router': nrm((N_SSD_LAYERS, D_MODEL, N_EXPERTS), D_MODEL ** -0.5),
        'b_router': nrm((N_SSD_LAYERS, N_EXPERTS), 0.01),
        'w_exp_gate': nrm((N_SSD_LAYERS, N_EXPERTS, D_MODEL, D_FF), D_MODEL ** -0.5),
        'w_exp_up': nrm((N_SSD_LAYERS, N_EXPERTS, D_MODEL, D_FF), D_MODEL ** -0.5),
        'w_exp_down': nrm((N_SSD_LAYERS, N_EXPERTS, D_FF, D_MODEL), D_FF ** -0.5),
        'g_final': gain((D_MODEL,)),
    }


def reference(x, mem, g_mix, g_mem, w_in_moba, w_in_ssd, w_mem_kv, w_out, rel_bias, conv_w, conv_b,
              dt_bias, a_log, d_skip, g_ssd_out, g_ffn, w_ffn_gate, w_ffn_up, w_ffn_down, w_router,
              b_router, w_exp_gate, w_exp_up, w_exp_down, g_final):
    for i in range(DEPTH):
        j = i // 2
        h = rms_norm(x, g_mix[i])
        mem_n = rms_norm(mem, g_mem[i])
        if i % 2 == 0:
            proj = h @ w_in_moba[j]
            y_tok = moba_attention(proj[..., :3 * MOBA_WIDTH], rel_bias)
            q_mem = proj[..., 3 * MOBA_WIDTH:]
        else:
            proj = h @ w_in_ssd[j]
            y_tok = ssd_mixer(proj[..., :IN_SSD - MEM_WIDTH], conv_w[j], conv_b[j], dt_bias[j],
                              a_log[j], d_skip[j], g_ssd_out[j])
            q_mem = proj[..., IN_SSD - MEM_WIDTH:]
        y_mem = memory_cross_attention(q_mem, mem_n, w_mem_kv[i])
        x = x + jnp.concatenate([y_tok, y_mem], axis=-1) @ w_out[i]
        h = rms_norm(x, g_ffn[i])
        if i % 2 == 0:
            x = x + swiglu(h, w_ffn_gate[j], w_ffn_up[j], w_ffn_down[j])
        else:
            x = x + moe_swiglu(h, w_router[j], b_router[j], w_exp_gate[j], w_exp_up[j], w_exp_down[j])
    return rms_norm(x, g_final)
```

```python
import math
from contextlib import ExitStack
import numpy as np
import concourse.bass as bass
import concourse.mybir as mybir
from concourse.bass_utils import run_bass_kernel_spmd

F32 = mybir.dt.float32
BF16 = mybir.dt.bfloat16
AF = mybir.ActivationFunctionType
ALU = mybir.AluOpType
AX = mybir.AxisListType

S = 2048
D = 1024
NT = 16
KC = 8
EPS = 1e-6
SCALE = 128 ** -0.5
NEG = -30000.0
BIG = 1.0e30
DFF = 3584
NFB = 7
NEXP = 8
DBG = None
PACE_N = 512


class R:
    __slots__ = ("w", "r")

    def __init__(self):
        self.w = None
        self.r = []


class Emitter:
    ENG = ("pe", "act", "dve", "pool", "sp")

    def __init__(self, nc, stack):
        self.nc = nc
        self.stack = stack
        self.q = {e: [] for e in self.ENG}
        self.sems = {}
        self.cnt = {}
        self.waited = {}
        for e in self.ENG:
            self._mksem(e)
        self.n_instr = 0

    def _mksem(self, key):
        self.sems[key] = self.stack.enter_context(self.nc.semaphore("s_" + key))
        self.cnt[key] = 0

    def sbuf(self, name, shape, dt):
        return self.stack.enter_context(self.nc.sbuf_tensor("sb_" + name, list(shape), dt))

    def psum(self, name, shape, dt=F32):
        return self.stack.enter_context(self.nc.psum_tensor("ps_" + name, list(shape), dt))

    def _deps(self, eng, reads, writes):
        deps = {}

        def add(t):
            if t is None:
                return
            k, v = t
            if k == "pe" and eng == "pe":
                return
            if deps.get(k, 0) < v:
                deps[k] = v
        for r in reads:
            add(r.w)
        for w in writes:
            add(w.w)
            for t in w.r:
                add(t)
        out = []
        for k, v in deps.items():
            if self.waited.get((eng, k), 0) >= v:
                continue
            self.waited[(eng, k)] = v
            out.append((self.sems[k], v))
        return out

    def _mark(self, ticket, reads, writes):
        for r in reads:
            r.r.append(ticket)
            if len(r.r) > 48:
                best = {}
                for k, v in r.r:
                    if best.get(k, 0) < v:
                        best[k] = v
                r.r = list(best.items())
        for w in writes:
            w.w = ticket
            w.r = []

    def op(self, eng, fn, reads=(), writes=(), inc=True):
        waits = self._deps(eng, reads, writes)
        ticket = (eng, self.cnt[eng] + 1)
        if inc:
            self.cnt[eng] += 1
        sem = self.sems[eng]
        self._mark(ticket, reads, writes)
        self.n_instr += 1

        def thunk(e):
            for s, v in waits:
                e.wait_ge(s, v)
            ins = fn(e)
            if inc:
                ins.then_inc(sem, 1)
        self.q[eng].append(thunk)
        return ticket

    def dma(self, queue, slot, out, in_, reads=(), writes=()):
        key = "d_" + slot + "_" + queue
        if key not in self.sems:
            self._mksem(key)
        waits = self._deps(queue, reads, writes)
        self.cnt[key] += 16
        ticket = (key, self.cnt[key])
        sem = self.sems[key]
        self._mark(ticket, reads, writes)
        self.n_instr += 1

        def thunk(e):
            for s, v in waits:
                e.wait_ge(s, v)
            e.dma_start(out=out, in_=in_).then_inc(sem, 16)
        self.q[queue].append(thunk)
        return ticket

    def barrier(self):
        for eng in self.ENG:
            waits = []
            for k, v in self.cnt.items():
                if v == 0 or k == eng and eng in ("pe",):
                    continue
                if self.waited.get((eng, k), 0) >= v:
                    continue
                self.waited[(eng, k)] = v
                waits.append((self.sems[k], v))
            if waits:
                def thunk(e, waits=waits):
                    for s, v in waits:
                        e.wait_ge(s, v)
                self.q[eng].append(thunk)

    def finish(self):
        nc = self.nc
        q = self.q
        with nc.Block() as block:
            @block.tensor
            def _(e):
                for f in q["pe"]:
                    f(e)

            @block.scalar
            def _(e):
                for f in q["act"]:
                    f(e)

            @block.vector
            def _(e):
                for f in q["dve"]:
                    f(e)

            @block.gpsimd
            def _(e):
                for f in q["pool"]:
                    f(e)

            @block.sync
            def _(e):
                for f in q["sp"]:
                    f(e)


def _t5_bucket_np(dist):
    n = np.maximum(dist, 0).astype(np.int32)
    max_exact = 16
    nf = np.maximum(n, 1).astype(np.float32)
    large = max_exact + (np.log(nf / np.float32(max_exact)) / np.float32(math.log(128 / max_exact))
                         * np.float32(32 - max_exact)).astype(np.int32)
    large = np.minimum(large, 31)
    return np.where(n < max_exact, n, large)


def _consts():
    c = {}
    i = np.arange(128)
    c["identf"] = np.eye(128, dtype=np.float32)
    c["U"] = (i[:, None] <= i[None, :]).astype(np.float32)
    c["SL"] = (i[:, None] > i[None, :]).astype(np.float32)
    c["causneg"] = np.where(i[:, None] > i[None, :], NEG, 0.0).astype(np.float32)
    E = np.zeros((8, 8 * 128), np.float32)
    for b in range(8):
        E[b, b * 128:(b + 1) * 128] = 1.0
    c["E"] = E
    tb = np.arange(16)[:, None] // 2
    bb = np.arange(8)[None, :]
    past = (bb < tb).astype(np.float32).reshape(1, 128)
    own = (bb == tb).astype(np.float32).reshape(1, 128)
    c["past01"] = np.repeat(past, 128, 0)
    c["own01"] = np.repeat(own, 128, 0)
    c["pastneg"] = np.repeat(np.where(past > 0, 0.0, -BIG).astype(np.float32), 128, 0)
    return c


def _feat_major(v):
    n = v.shape[0] // 128
    return np.ascontiguousarray(v.reshape(n, 128).T)


def _bcast(v):
    return np.ascontiguousarray(np.broadcast_to(v.reshape(1, -1), (128, v.size)))


def _kmajor(w):
    k, n = w.shape
    return np.ascontiguousarray(w.reshape(k // 128, 128, n).transpose(1, 0, 2))


def _prep_shared(inp):
    f = np.float32
    sh = {}
    w0 = inp["w_in_moba"][0]
    wq = _kmajor(w0[:, 0:1536]).reshape(128, 8, 12, 128)
    wk = _kmajor(w0[:, 1536:3072]).reshape(128, 8, 12, 128)
    wv = _kmajor(w0[:, 3072:4608]).reshape(128, 8, 12, 128)
    wqkv = np.stack([wq, wk, wv], axis=3)
    sh["wqkv0"] = np.ascontiguousarray(wqkv.transpose(2, 0, 1, 3, 4)).reshape(12, 128, 8 * 384)
    wqm = _kmajor(w0[:, 4608:5120]).reshape(128, 8, 4, 128)
    sh["wqm0"] = np.ascontiguousarray(wqm.transpose(2, 0, 1, 3)).reshape(4, 128, 8 * 128)
    sh["wkv"] = np.stack([_kmajor(inp["w_mem_kv"][i]) for i in range(2)]).reshape(2, 128, 8 * 1024)
    sh["wout"] = np.stack([_kmajor(inp["w_out"][i]) for i in range(2)]).reshape(2, 128, 16 * 1024)
    wg = np.concatenate([inp["w_ffn_gate"], inp["w_exp_gate"][0]], 0)
    wu = np.concatenate([inp["w_ffn_up"], inp["w_exp_up"][0]], 0)
    wd = np.concatenate([inp["w_ffn_down"], inp["w_exp_down"][0]], 0)
    wgk = wg.reshape(9, 8, 128, NFB, 512).transpose(0, 3, 2, 1, 4)
    wuk = wu.reshape(9, 8, 128, NFB, 512).transpose(0, 3, 2, 1, 4)
    sh["wgu"] = np.ascontiguousarray(np.stack([wgk, wuk], axis=4)).reshape(9, NFB, 128, 8 * 2 * 512)
    sh["wd"] = np.ascontiguousarray(wd.reshape(9, NFB, 4, 128, 1024).transpose(0, 1, 3, 2, 4)).reshape(9, NFB, 128, 4 * 1024)
    w1 = inp["w_in_ssd"][0]
    blocks = [w1[:, j * 512:(j + 1) * 512] for j in range(8)] + [w1[:, 4120:4632]]
    sh["wssd"] = np.stack([_kmajor(b) for b in blocks]).reshape(9, 128, 8 * 512)
    sh["wdt"] = _kmajor(w1[:, 4096:4120]).reshape(128, 8 * 24)
    gv = np.stack([_feat_major(inp["g_mix"][0]), _feat_major(inp["g_mix"][1]),
                   _feat_major(inp["g_ffn"][0]), _feat_major(inp["g_ffn"][1]),
                   _feat_major(inp["g_mem"][0]), _feat_major(inp["g_mem"][1])], axis=1)
    sh["gvec"] = np.ascontiguousarray(gv).reshape(128, 48)
    sh["gfin"] = _bcast(inp["g_final"])
    cw = inp["conv_w"][0]
    sh["convw"] = np.ascontiguousarray(cw.reshape(4, 20, 128).transpose(2, 1, 0)).reshape(128, 80)
    sh["convb"] = _feat_major(inp["conv_b"][0])
    sh["dtb"] = _bcast(inp["dt_bias"][0])
    sh["alog"] = _bcast(inp["a_log"][0])
    sh["dskip"] = _bcast(inp["d_skip"][0])
    sh["gssd"] = _bcast(inp["g_ssd_out"][0])
    sh["wr"] = _kmajor(inp["w_router"][0]).reshape(128, 64)
    sh["br"] = _bcast(inp["b_router"][0])
    rb = inp["rel_bias"]
    i = np.arange(128)
    d0 = i[None, :] - i[:, None]
    bk0 = _t5_bucket_np(d0)
    bk1 = _t5_bucket_np(d0 + 128)
    tt = np.stack([rb[bk0], rb[bk1]], 0)
    sh["ttg"] = np.ascontiguousarray(tt.transpose(1, 0, 3, 2)).reshape(128, 2 * 12 * 128)
    sh["b31"] = _bcast(rb[31])
    sh.update(_consts())
    return {k: np.ascontiguousarray(v, dtype=f) for k, v in sh.items()}


def build(n_layers=2, n_seq=2):
    nc = bass.Bass("TRN2", target_bir_lowering=False)

    def din(name, shape):
        return nc.dram_tensor(name, list(shape), F32, kind="ExternalInput").ap()

    x_d = din("x", [n_seq, S, D])
    mem_d = din("mem", [n_seq, 256, D])
    wqkv0_d = din("wqkv0", [12, 128, 8 * 384])
    wqm0_d = din("wqm0", [4, 128, 8 * 128])
    wkv_d = din("wkv", [2, 128, 8 * 1024])
    wout_d = din("wout", [2, 128, 16 * 1024])
    wgu_d = din("wgu", [9, NFB, 128, 8 * 2 * 512])
    wd_d = din("wd", [9, NFB, 128, 4 * 1024])
    wssd_d = din("wssd", [9, 128, 8 * 512])
    wdt_d = din("wdt", [128, 8 * 24])
    gvec_d = din("gvec", [128, 48])
    gfin_d = din("gfin", [128, D])
    convw_d = din("convw", [128, 80])
    convb_d = din("convb", [128, 20])
    dtb_d = din("dtb", [128, 24])
    alog_d = din("alog", [128, 24])
    dskip_d = din("dskip", [128, 24])
    gssd_d = din("gssd", [128, 1536])
    wr_d = din("wr", [128, 64])
    br_d = din("br", [128, 8])
    ttg_d = din("ttg", [128, 2 * 12 * 128])
    b31_d = din("b31", [128, 12])
    identf_d = din("identf", [128, 128])
    U_d = din("U", [128, 128])
    SL_d = din("SL", [128, 128])
    causneg_d = din("causneg", [128, 128])
    E_d = din("E", [8, 1024])
    past01_d = din("past01", [128, 128])
    own01_d = din("own01", [128, 128])
    pastneg_d = din("pastneg", [128, 128])
    out_d = nc.dram_tensor("out", [n_seq, S, D], F32, kind="ExternalOutput").ap()

    with ExitStack() as st:
        em = Emitter(nc, st)

        def MM(out, lhsT, rhs, start, stop, reads, writes, inc=None):
            if inc is None:
                inc = stop
            em.op("pe", lambda e: e.matmul(out, lhsT=lhsT, rhs=rhs, start=start, stop=stop),
                  reads, writes, inc)

        def TR(out, in_, ident, reads, writes, inc=True):
            em.op("pe", lambda e: e.transpose(out=out, in_=in_, identity=ident), reads, writes, inc)

        def ACT(out, in_, func, reads, writes, bias=None, scale=None, accum_out=None):
            kw = {}
            if bias is not None:
                kw["bias"] = bias
            if scale is not None:
                kw["scale"] = scale
            if accum_out is not None:
                kw["accum_out"] = accum_out
            em.op("act", lambda e: e.activation(out=out, in_=in_, func=func, **kw), reads, writes)

        def TT(out, in0, in1, op, reads, writes, eng="dve"):
            em.op(eng, lambda e: e.tensor_tensor(out=out, in0=in0, in1=in1, op=op), reads, writes)

        def TS(out, in0, s1, s2, op0, op1=None, reads=(), writes=(), eng="dve", accum_out=None):
            kw = {}
            if op1 is not None:
                kw["op1"] = op1
            if accum_out is not None:
                kw["accum_out"] = accum_out
            em.op(eng, lambda e: e.tensor_scalar(out=out, in0=in0, scalar1=s1, scalar2=s2, op0=op0, **kw),
                  reads, writes)

        def STT(out, in0, scalar, in1, op0, op1, reads, writes, eng="dve"):
            em.op(eng, lambda e: e.scalar_tensor_tensor(out=out, in0=in0, scalar=scalar, in1=in1,
                                                        op0=op0, op1=op1), reads, writes)

        def CP(out, in_, reads, writes, eng="dve"):
            em.op(eng, lambda e: e.tensor_copy(out=out, in_=in_), reads, writes)

        def RED(out, in_, op, reads, writes, eng="dve"):
            em.op(eng, lambda e: e.tensor_reduce(out=out, in_=in_, axis=AX.X, op=op), reads, writes)

        def RECIP(out, in_, reads, writes):
            em.op("dve", lambda e: e.reciprocal(out=out, in_=in_), reads, writes)

        def MEMSET(ap, val, writes, eng="pool", reads=()):
            em.op(eng, lambda e: e.memset(ap, val), reads, writes)

        x_sb = em.sbuf("x_sb", [128, NT, D], F32)
        Rx = [R() for _ in range(NT)]
        RhT = [R() for _ in range(NT)]

        def cload(name, src, shape, dt=F32, queue=None):
            t = em.sbuf(name, shape, dt)
            r = R()
            q = queue or ("pool" if dt != F32 else "sp")
            flat = t[:] if len(shape) == 2 else t[:]
            em.dma(q, "const", flat, src, writes=[r])
            return t, r

        identb, Rident = cload("identb", identf_d, [128, 128], BF16)
        identf, Ridentf = cload("identf", identf_d, [128, 128])
        U_sb, RU = cload("U_sb", U_d, [128, 128])
        SL_sb, RSL = cload("SL_sb", SL_d, [128, 128])
        causneg, Rcaus = cload("causneg", causneg_d, [128, 128])
        E_sb, RE = cload("E_sb", E_d, [8, 1024], BF16)
        past01, Rpast = cload("past01", past01_d, [128, 128])
        own01, Rown = cload("own01", own01_d, [128, 128])
        pastneg, Rpastneg = cload("pastneg", pastneg_d, [128, 128])
        gvec, Rgvec = cload("gvec", gvec_d, [128, 48])
        gfin, Rgfin = cload("gfin", gfin_d, [128, D])
        b31, Rb31 = cload("b31", b31_d, [128, 12])
        onesb = em.sbuf("onesb", [128, 128], BF16)
        Rones = R()
        MEMSET(onesb[:], 1.0, [Rones])
        onesf = em.sbuf("onesf", [128, 128], F32)
        Ronesf = R()
        MEMSET(onesf[:], 1.0, [Ronesf])
        junk = em.sbuf("junk", [128, D], BF16)
        epsc = em.sbuf("epsc", [128, 1], F32)
        Reps = R()
        MEMSET(epsc[:], EPS, [Reps])
        ss = em.sbuf("ss", [128, NT], F32)
        rstd = em.sbuf("rstd", [128, NT], F32)
        Rss = [R() for _ in range(NT)]
        Rrstd = [R() for _ in range(NT)]
        xn = [em.sbuf("xn%d" % i, [128, D], BF16) for i in range(2)]
        Rxn = [R(), R()]

        wdt_sb, Rwdt = cload("wdt_sb", wdt_d, [128, 192], BF16)
        convw, Rconvw = cload("convw_sb", convw_d, [128, 80])
        convb, Rconvb = cload("convb_sb", convb_d, [128, 20])
        dtb, Rdtb = cload("dtb_sb", dtb_d, [128, 24])
        a_bc, Ra_bc = cload("a_bc", alog_d, [128, 24])
        dskip, Rdskip = cload("dskip_sb", dskip_d, [128, 24])
        wr_sb, Rwr = cload("wr_sb", wr_d, [128, 64], BF16)
        br_sb, Rbr = cload("br_sb", br_d, [128, 8])
        gw = em.sbuf("gw", [128, 128], F32)
        Rgw = R()
        onec = em.sbuf("onec", [128, 1], F32)
        Ronec = R()
        MEMSET(onec[:], 1.0, [Ronec])

        TTb = em.sbuf("TTb", [128, 2 * 12 * 128], BF16)
        RTTb = R()

        pf = [em.psum("pf%d" % i, [128, 512], F32) for i in range(6)]
        Rpf = [R() for _ in range(6)]
        pb = [em.psum("pb%d" % i, [128, 1024], BF16) for i in range(2)]
        Rpb = [R(), R()]

        ARENA = 86 * 1024
        ARENA_FULL = ARENA + 32 * 1024
        arena = em.sbuf("arena", [128, ARENA_FULL // 2], BF16)
        hT = arena[:, ARENA // 2: ARENA_FULL // 2].rearrange("p (a b) -> p a b", b=S)

        class Carver:
            def __init__(self, limit=None):
                self.off = 0
                self.limit = limit or ARENA

            def take(self, shape, dt, parts=128):
                esz = 4 if dt == F32 else 2
                n = int(np.prod(shape[1:]))
                nbytes = (n * esz + 31) // 32 * 32
                assert self.off + nbytes <= self.limit, ("arena overflow", self.off, nbytes)
                ap = arena[0:shape[0], self.off // 2: self.off // 2 + n * esz // 2]
                if dt == F32:
                    ap = ap.bitcast(F32)
                self.off += nbytes
                if len(shape) == 3:
                    ap = ap.rearrange("p (a b) -> p a b", b=shape[2])
                elif len(shape) == 4:
                    ap = ap.rearrange("p (a b c) -> p a b c", b=shape[2], c=shape[3])
                return ap

        def setup_bias():
            cv = Carver()
            ttg = cv.take([128, 2 * 12 * 128], F32)
            Rttg = R()
            em.dma("sp", "const", ttg, ttg_d, writes=[Rttg])
            em.barrier()
            ACT(a_bc[:], a_bc[:], AF.Exp, [Ra_bc], [Ra_bc])
            TS(a_bc[:], a_bc[:], -1.0, None, ALU.mult, reads=[Ra_bc], writes=[Ra_bc])
            for j in range(2):
                for h in range(12):
                    sl = slice((j * 12 + h) * 128, (j * 12 + h + 1) * 128)
                    if j == 0:
                        STT(TTb[:, sl], ttg[:, sl], b31[:, h:h + 1], causneg[:], ALU.subtract, ALU.add,
                            [Rttg, Rb31, Rcaus], [RTTb])
                    else:
                        TS(TTb[:, sl], ttg[:, sl], b31[:, h:h + 1], None, ALU.subtract,
                           reads=[Rttg, Rb31], writes=[RTTb])
            em.barrier()

        setup_bias()

        nrm_ctr = [0]

        def norm_tile(src_ap, Rsrc, gi, dst_ap, Rdst, ss_ap, rstd_ap, Rs, Rr, ncols=D):
            i = nrm_ctr[0] % 2
            nrm_ctr[0] += 1
            MEMSET(ss_ap, 0.0, [Rs])
            ACT(junk[:], src_ap, AF.Square, [Rsrc, Rs], [Rs], accum_out=ss_ap)
            ACT(rstd_ap, ss_ap, AF.Sqrt, [Rs, Reps], [Rr], bias=epsc[:, 0:1], scale=1.0 / D)
            RECIP(rstd_ap, rstd_ap, [Rr], [Rr])
            TS(xn[i][:], src_ap, rstd_ap, None, ALU.mult, reads=[Rsrc, Rr], writes=[Rxn[i]])
            for kc in range(KC):
                TR(pb[i][:, kc * 128:(kc + 1) * 128], xn[i][:, kc * 128:(kc + 1) * 128], identb[:],
                   [Rxn[i], Rident], [Rpb[i]], inc=(kc == KC - 1))
            gb = gvec[:, gi * 8:(gi + 1) * 8].unsqueeze(2).to_broadcast([128, 8, 128])
            TT(dst_ap, pb[i][:].rearrange("p (a b) -> p a b", b=128), gb, ALU.mult,
               [Rpb[i], Rgvec], [Rdst])

        def norm_seq(gi):
            for t in range(NT):
                norm_tile(x_sb[:, t, :], Rx[t], gi, hT[:, :, t * 128:(t + 1) * 128], RhT[t],
                          ss[:, t:t + 1], rstd[:, t:t + 1], Rss[t], Rrstd[t])

        pT_ctr = [0]
        s_ctr = [0]

        def attention_head(qT_ap, RqT, kT_fn, RkT, V_fn, RV, n_q, out_fn, Rout, pTs, RpTs, rl, Rrl,
                           moba=None):
            O, L = pf[4], pf[5]
            RO, RL = Rpf[4], Rpf[5]
            ngrp = (n_q + 511) // 512
            pending = None

            def emit_pv(kt, c0, qn, pi, first, last):
                MM(O[:, c0:qn], V_fn(kt), pTs[pi][:, c0:qn], first, last, [RV, RpTs[pi]], [RO], inc=False)
                MM(L[:, c0:qn], onesb[:], pTs[pi][:, c0:qn], first, last, [Rones, RpTs[pi]], [RL], inc=True)

            for qg in range(ngrp):
                q0 = qg * 512
                qn = min(512, n_q - q0)
                if moba is None:
                    kts = [0, 1]
                else:
                    kts = list(range(0, 4 * qg + 4))
                for idx, kt in enumerate(kts):
                    if moba is None:
                        c0 = 0
                    else:
                        c0 = max(0, kt - 4 * qg) * 128
                    si = s_ctr[0] % 2
                    s_ctr[0] += 1
                    Sb, RS = pf[2 + si], Rpf[2 + si]
                    extra = []
                    if moba is not None:
                        b = kt // 2
                        extra.append((E_sb[0:8, b * 128:(b + 1) * 128], moba["MnegT"][0:8, q0 + c0:q0 + qn],
                                      c0, qn, [RE, moba["RM"]]))
                        for j in range(2):
                            qt = kt + j
                            cc = (qt - 4 * qg) * 128
                            if 0 <= cc < qn and cc >= c0:
                                sl = slice((j * 12 + moba["h"]) * 128, (j * 12 + moba["h"] + 1) * 128)
                                extra.append((identb[:], TTb[:, sl], cc, cc + 128, [Rident, RTTb]))
                    MM(Sb[:, c0:qn], kT_fn(kt), qT_ap[:, q0 + c0:q0 + qn], True, len(extra) == 0,
                       [RkT, RqT], [RS])
                    for ei, (l_, r_, a0, a1, rr) in enumerate(extra):
                        MM(Sb[:, a0:a1], l_, r_, False, ei == len(extra) - 1, rr, [RS])
                    pi = pT_ctr[0] % len(pTs)
                    pT_ctr[0] += 1
                    ACT(pTs[pi][:, c0:qn], Sb[:, c0:qn], AF.Exp, [RS], [RpTs[pi]])
                    if pending is not None:
                        emit_pv(*pending)
                    pending = (kt, c0, qn, pi, idx == 0, idx == len(kts) - 1)
                emit_pv(*pending)
                pending = None
                RECIP(rl[:, 0:qn], L[:, 0:qn], [RL], [Rrl])
                TT(out_fn(q0, q0 + qn), O[:, 0:qn], rl[:, 0:qn], ALU.mult, [RO, Rrl], [Rout])

        def mem_kv(s, layer, cv, kTm, RkTm, vm, Rvm):
            memx = cv.take([128, 2, D], F32)
            Rmemx = [R(), R()]
            memnT = cv.take([128, KC, 256], BF16)
            RmemnT = [R(), R()]
            wkv = cv.take([128, KC, 1024], BF16)
            Rwkv = R()
            mss = cv.take([128, 2], F32)
            mrs = cv.take([128, 2], F32)
            Rm1 = [R(), R()]
            Rm2 = [R(), R()]
            em.dma("pool", "wkv", wkv.rearrange("p a b -> p (a b)"), wkv_d[layer], writes=[Rwkv])
            for mt in range(2):
                em.dma("sp", "memx%d" % mt, memx[:, mt, :], mem_d[s, mt * 128:(mt + 1) * 128, :], writes=[Rmemx[mt]])
            for mt in range(2):
                norm_tile(memx[:, mt, :], Rmemx[mt], 4 + layer, memnT[:, :, mt * 128:(mt + 1) * 128], RmemnT[mt],
                          mss[:, mt:mt + 1], mrs[:, mt:mt + 1], Rm1[mt], Rm2[mt])
            for hm in range(4):
                ps, Rp = pf[hm % 2], Rpf[hm % 2]
                for kc in range(KC):
                    MM(ps[:, 0:256], wkv[:, kc, hm * 128:(hm + 1) * 128], memnT[:, kc, :], kc == 0, kc == KC - 1,
                       [Rwkv] + RmemnT, [Rp])
                CP(kTm[:, hm, :], ps[:, 0:256], [Rp], [RkTm])
            for mt in range(2):
                ps, Rp = pf[mt % 2], Rpf[mt % 2]
                for kc in range(KC):
                    MM(ps[:, :], memnT[:, kc, mt * 128:(mt + 1) * 128], wkv[:, kc, 512:1024], kc == 0, kc == KC - 1,
                       [Rwkv, RmemnT[mt]], [Rp])
                ACT(vm[:, mt, :], ps[:, :], AF.Copy, [Rp], [Rvm])

        def mixer_moba(s):
            cv = Carver()
            kTm = cv.take([128, 4, 256], BF16)
            RkTm = R()
            vm = cv.take([128, 2, 512], BF16)
            Rvm = R()
            mark = cv.off
            mem_kv(s, 0, cv, kTm, RkTm, vm, Rvm)
            em.barrier()
            cv.off = mark
            wq = [cv.take([128, KC, 384], BF16) for _ in range(2)]
            Rwq = [R(), R()]
            qT = [cv.take([128, S], BF16) for _ in range(2)]
            RqT = [R(), R()]
            kT = [cv.take([128, S], BF16) for _ in range(2)]
            RkT = [R(), R()]
            Vh = [cv.take([128, NT, 128], BF16) for _ in range(2)]
            RV = [R(), R()]
            oTg = cv.take([128, 4, S], BF16)
            RoTg = [R() for _ in range(4)]
            woutg = cv.take([128, 4, D], BF16)
            Rwoutg = R()
            pTs = [cv.take([128, 512], BF16) for _ in range(3)]
            RpTs = [R() for _ in range(3)]
            rl = cv.take([128, 512], F32)
            Rrl = R()
            gm = cv.take([128, 128], F32)
            gm2 = cv.take([128, 128], F32)
            eq = cv.take([128, 128], F32)
            mx = cv.take([128, 16], F32)
            Rg = R()
            Mneg = cv.take([128, 128], BF16)
            RMneg = R()
            MnegT = cv.take([8, S], BF16, parts=8)
            RMnegT = R()
            km = cv.take([128, 8], F32)
            kmb = cv.take([128, 8], BF16)
            Rkm = R()

            def load_w(h):
                sl = h % 2
                if h < 12:
                    em.dma("pool", "wq%d" % sl, wq[sl].rearrange("p a b -> p (a b)"), wqkv0_d[h], writes=[Rwq[sl]])
                else:
                    em.dma("pool", "wq%d" % sl, wq[sl][:, :, 0:128], wqm0_d[h - 12].rearrange("p (a b) -> p a b", b=128),
                           writes=[Rwq[sl]])

            load_w(0)
            pj = [0]

            def proj_fm(dst, Rdst, w, Rw, c0, scale, use_act):
                for tg in range(4):
                    i = pj[0] % 2
                    pj[0] += 1
                    ps, Rp = pf[i], Rpf[i]
                    for kc in range(KC):
                        MM(ps[:, :], w[:, kc, c0:c0 + 128], hT[:, kc, tg * 512:(tg + 1) * 512], kc == 0, kc == KC - 1,
                           [Rw] + RhT[tg * 4:tg * 4 + 4], [Rp])
                    if use_act:
                        ACT(dst[:, tg * 512:(tg + 1) * 512], ps[:, :], AF.Copy, [Rp], [Rdst], scale=scale)
                    else:
                        CP(dst[:, tg * 512:(tg + 1) * 512], ps[:, :], [Rp], [Rdst])

            for h in range(16):
                sl = h % 2
                grp, hh = h // 4, h % 4
                if h + 1 < 16:
                    load_w(h + 1)
                if hh == 0:
                    em.dma("pool", "woutg", woutg.rearrange("p a b -> p (a b)"),
                           wout_d[0][:, grp * 4096:(grp + 1) * 4096], writes=[Rwoutg])
                proj_fm(qT[sl], RqT[sl], wq[sl], Rwq[sl], 0, SCALE, True)
                if h < 12:
                    proj_fm(kT[sl], RkT[sl], wq[sl], Rwq[sl], 128, None, False)
                    for t4 in range(4):
                        i = pj[0] % 2
                        pj[0] += 1
                        ps, Rp = pf[i], Rpf[i]
                        for j in range(4):
                            t = t4 * 4 + j
                            for kc in range(KC):
                                MM(ps[:, j * 128:(j + 1) * 128], hT[:, kc, t * 128:(t + 1) * 128], wq[sl][:, kc, 256:384],
                                   kc == 0, kc == KC - 1, [Rwq[sl], RhT[t]], [Rp], inc=(kc == KC - 1 and j == 3))
                        ACT(Vh[sl][:, t4 * 4:(t4 + 1) * 4, :], ps[:].rearrange("p (a b) -> p a b", b=128), AF.Copy,
                            [Rp], [RV[sl]])
                    RED(km[:], kT[sl].rearrange("p (a b) -> p a b", b=256), ALU.add, [RkT[sl]], [Rkm])
                    TS(kmb[:], km[:], 1.0 / 256, None, ALU.mult, reads=[Rkm], writes=[Rkm])
                    G, RG = pf[0], Rpf[0]
                    pj[0] = 1
                    for t in range(NT):
                        MM(G[:, t * 8:(t + 1) * 8], qT[sl][:, t * 128:(t + 1) * 128], kmb[:], True, True,
                           [RqT[sl], Rkm], [RG], inc=(t == NT - 1))
                    g3 = lambda a: a.rearrange("p (a b) -> p a b", b=8)
                    mb = lambda: mx[:].unsqueeze(2).to_broadcast([128, 16, 8])
                    TT(gm[:], G[:, 0:128], pastneg[:], ALU.add, [RG, Rpastneg], [Rg])
                    RED(mx[:], g3(gm[:]), ALU.max, [Rg], [Rg])
                    TT(g3(eq[:]), g3(gm[:]), mb(), ALU.is_equal, [Rg], [Rg])
                    STT(gm2[:], eq[:], -BIG, gm[:], ALU.mult, ALU.add, [Rg], [Rg])
                    RED(mx[:], g3(gm2[:]), ALU.max, [Rg], [Rg])
                    TT(g3(eq[:]), g3(gm2[:]), mb(), ALU.is_equal, [Rg], [Rg])
                    STT(gm2[:], eq[:], -BIG, gm2[:], ALU.mult, ALU.add, [Rg], [Rg])
                    RED(mx[:], g3(gm2[:]), ALU.max, [Rg], [Rg])
                    TT(g3(eq[:]), g3(gm[:]), mb(), ALU.is_ge, [Rg], [Rg])
                    TT(eq[:], eq[:], past01[:], ALU.mult, [Rg, Rpast], [Rg])
                    TT(eq[:], eq[:], own01[:], ALU.add, [Rg, Rown], [Rg])
                    TS(Mneg[:], eq[:], -1.0, -NEG, ALU.add, ALU.mult, reads=[Rg], writes=[RMneg])
                    for half in range(2):
                        pbi = half
                        for t8 in range(8):
                            t = half * 8 + t8
                            TR(pb[pbi][0:8, t8 * 128:(t8 + 1) * 128], Mneg[:, t * 8:(t + 1) * 8], identb[:],
                               [RMneg, Rident], [Rpb[pbi]], inc=(t8 == 7))
                        ACT(MnegT[0:8, half * 1024:(half + 1) * 1024], pb[pbi][0:8, :], AF.Copy, [Rpb[pbi]], [RMnegT])
                    attention_head(qT[sl], RqT[sl],
                                   lambda kt, sl=sl: kT[sl][:, kt * 128:(kt + 1) * 128], RkT[sl],
                                   lambda kt, sl=sl: Vh[sl][:, kt, :], RV[sl], S,
                                   lambda a, b_, hh=hh: oTg[:, hh, a:b_], RoTg[hh], pTs, RpTs, rl, Rrl,
                                   moba=dict(h=h, MnegT=MnegT, RM=RMnegT))
                else:
                    hm = h - 12
                    attention_head(qT[sl], RqT[sl],
                                   lambda kt, hm=hm: kTm[:, hm, kt * 128:(kt + 1) * 128], RkTm,
                                   lambda kt, hm=hm: vm[:, kt, hm * 128:(hm + 1) * 128], Rvm, S,
                                   lambda a, b_, hh=hh: oTg[:, hh, a:b_], RoTg[hh], pTs, RpTs, rl, Rrl, moba=None)
                if hh == 3:
                    for t in range(NT):
                        for half in range(2):
                            i = pj[0] % 2
                            pj[0] += 1
                            ps, Rp = pf[i], Rpf[i]
                            for c in range(4):
                                MM(ps[:, :], oTg[:, c, t * 128:(t + 1) * 128], woutg[:, c, half * 512:(half + 1) * 512],
                                   c == 0, c == 3, [RoTg[c], Rwoutg], [Rp])
                            TT(x_sb[:, t, half * 512:(half + 1) * 512], x_sb[:, t, half * 512:(half + 1) * 512], ps[:, :],
                               ALU.add, [Rp, Rx[t]], [Rx[t]])
            em.barrier()

        ffn_state = {"n": 0}

        def ffn_phase(experts, gw=None, Rgw=None):
            cv = Carver()
            wgu = [cv.take([128, KC, 2, 512], BF16) for _ in range(2)]
            wdn = [cv.take([128, 4, D], BF16) for _ in range(2)]
            Rw = [R(), R()]
            act = cv.take([128, 4, S], BF16)
            Ract = [R() for _ in range(4)]
            sg = [cv.take([128, 512], F32) for _ in range(2)]
            Rsg = [R(), R()]
            units = [(e, fb) for e in experts for fb in range(NFB)]
            pace_buf = cv.take([128, max(PACE_N, 8)], F32)
            Rpace = R()

            def load(u):
                e, fb = units[u]
                sl = u % 2
                em.dma("pool", "wgu%d" % sl, wgu[sl].rearrange("p a b c -> p (a b c)"), wgu_d[e, fb], writes=[Rw[sl]])
                em.dma("pool", "wgu%d" % sl, wdn[sl].rearrange("p a b -> p (a b)"), wd_d[e, fb], writes=[Rw[sl]])

            load(0)
            ctr = 0
            for u, (e, fb) in enumerate(units):
                sl = u % 2
                if u + 1 < len(units):
                    load(u + 1)
                for tg in range(4):
                    for fc in range(4):
                        i = ctr % 2
                        ctr += 1
                        gp, Rgp = pf[i], Rpf[i]
                        up, Rup = pf[2 + i], Rpf[2 + i]
                        rds = [Rw[sl]] + RhT[tg * 4:tg * 4 + 4] + ([Rpace] if PACE_N else [])
                        for kc in range(KC):
                            MM(gp[:, :], wgu[sl][:, kc, 0, fc * 128:(fc + 1) * 128], hT[:, kc, tg * 512:(tg + 1) * 512],
                               kc == 0, kc == KC - 1, rds, [Rgp])
                        for kc in range(KC):
                            MM(up[:, :], wgu[sl][:, kc, 1, fc * 128:(fc + 1) * 128], hT[:, kc, tg * 512:(tg + 1) * 512],
                               kc == 0, kc == KC - 1, rds, [Rup])
                        if PACE_N:
                            MEMSET(pace_buf[:, 0:PACE_N], 0.0, [Rpace], reads=[Rgp, Rup])
                        ACT(sg[i][:], gp[:, :], AF.Silu, [Rgp], [Rsg[i]])
                        TT(act[:, fc, tg * 512:(tg + 1) * 512], sg[i][:], up[:, :], ALU.mult, [Rsg[i], Rup], [Ract[fc]])
                for t in range(NT):
                    for half in range(2):
                        i = ctr % 2
                        ctr += 1
                        yp, Ryp = pf[4 + i], Rpf[4 + i]
                        pace_here = PACE_N and (t * 2 + half) % 4 == 0
                        for fc in range(4):
                            MM(yp[:, :], act[:, fc, t * 128:(t + 1) * 128], wdn[sl][:, fc, half * 512:(half + 1) * 512],
                               fc == 0, fc == 3, [Ract[fc], Rw[sl]] + ([Rpace] if pace_here else []), [Ryp])
                        if PACE_N and (t * 2 + half) % 4 == 3:
                            MEMSET(pace_buf[:, 0:PACE_N], 0.0, [Rpace], reads=[Ryp])
                        xs_ = x_sb[:, t, half * 512:(half + 1) * 512]
                        if gw is None:
                            TT(xs_, xs_, yp[:, :], ALU.add, [Ryp, Rx[t]], [Rx[t]])
                        else:
                            STT(xs_, yp[:, :], gw[:, t, e - 1:e], xs_, ALU.mult, ALU.add, [Ryp, Rx[t], Rgw], [Rx[t]])
            em.barrier()

        def mixer_ssd(s):
            cv = Carver(ARENA_FULL)
            kTm = cv.take([128, 4, 256], BF16)
            RkTm = R()
            vm = cv.take([128, 2, 512], BF16)
            Rvm = R()
            mark = cv.off
            mem_kv(s, 1, cv, kTm, RkTm, vm, Rvm)
            em.barrier()
            cv.off = mark
            wblk = [cv.take([128, 4096], BF16) for _ in range(3)]
            Rwblk = [R() for _ in range(3)]
            hTc = [cv.take([128, KC, 128], BF16) for _ in range(2)]
            RhTc = [R(), R()]
            sz = cv.take([128, 1536], F32)
            Rsz = R()
            pre = [cv.take([128, 4, 131], F32) for _ in range(2)]
            Rpre = [R(), R()]
            halo = cv.take([128, 20, 3], F32)
            Rhalo = [R() for _ in range(5)]
            cacc = [cv.take([128, 128], F32) for _ in range(2)]
            Rcacc = [R(), R()]
            csf = [cv.take([128, 128], F32) for _ in range(2)]
            Rcsf = [R(), R()]
            xs_tok = cv.take([128, 1536], F32)
            Rxs = R()
            BT = cv.take([128, 4, 128], BF16)
            RBT = R()
            CT = cv.take([128, 4, 128], BF16)
            RCT = R()
            Btok = cv.take([128, 512], BF16)
            RBtok = R()
            dtv = cv.take([128, 24], F32)
            la = cv.take([128, 24], F32)
            lacs = cv.take([128, 24], F32)
            tot = cv.take([128, 24], F32)
            expcs = cv.take([128, 24], F32)
            dte = cv.take([128, 24], F32)
            cdec = cv.take([128, 24], F32)
            Rdt = R()
            xdt = cv.take([128, 1536], BF16)
            Rxdt = R()
            xdte = cv.take([128, 1536], BF16)
            Rxdte = R()
            lh = [cv.take([128, 128], F32) for _ in range(4)]
            Rlh = [R() for _ in range(4)]
            dec = [cv.take([128, 512], F32) for _ in range(2)]
            Rdec = [R(), R()]
            scT = [cv.take([128, 4, 128], BF16) for _ in range(2)]
            RscT = [R(), R()]
            CBm = [cv.take([128, 128], F32) for _ in range(2)]
            RCBm = [R(), R()]
            hst = cv.take([128, 4, 384], F32)
            hbf = cv.take([128, 4, 384], BF16)
            Rhst = [R() for _ in range(4)]
            Rhbf = [R() for _ in range(4)]
            y_sb = cv.take([128, 1536], F32)
            Ry = R()
            ytmp = cv.take([128, 1536], F32)
            Rytmp = R()
            yoff = cv.take([128, 384], F32)
            Ryoff = R()
            gss = cv.take([128, 4], F32)
            Rgss = R()
            un = cv.take([128, 1536], BF16)
            Run = R()
            ycT = cv.take([128, 16, 128], BF16)
            RycT = [R() for _ in range(16)]
            qmT = cv.take([128, 4, 128], BF16)
            RqmT = R()
            pTs = [cv.take([128, 512], BF16) for _ in range(3)]
            RpTs = [R() for _ in range(3)]
            rl = cv.take([128, 512], F32)
            Rrl = R()
            gssd = cv.take([128, 1536], F32)
            Rgssd = R()
            em.dma("sp", "gssd", gssd, gssd_d, writes=[Rgssd])
            MEMSET(halo.rearrange("p a b -> p (a b)"), 0.0, Rhalo)
            MEMSET(hst.rearrange("p a b -> p (a b)"), 0.0, Rhst)
            MEMSET(hbf.rearrange("p a b -> p (a b)"), 0.0, Rhbf)

            NU = 13

            def load_unit(u):
                c, j = divmod(u, NU)
                if c >= NT:
                    return
                sl = u % 3
                em.dma("sp", "wblk%d" % sl, wblk[sl], wl1_bf[j], reads=[Rwl1[j]], writes=[Rwblk[sl]])

            load_unit(0)
            load_unit(1)
            pj = [0]
            tc = [0]
            norm_tile(x_sb[:, 0, :], Rx[0], 1, hTc[0], RhTc[0], ss[:, 0:1], rstd[:, 0:1], Rss[0], Rrstd[0])
            for c in range(NT):
                ci = c % 2
                hc = hTc[ci]
                Rhc = RhTc[ci]
                P2, RP2 = pf[2], Rpf[2]
                for kc in range(KC):
                    MM(P2[:, 0:24], hc[:, kc, :], wdt_sb[:, kc * 24:(kc + 1) * 24], kc == 0, kc == KC - 1, [Rhc, Rwdt], [RP2])
                TT(dtv[:], P2[:, 0:24], dtb[:], ALU.add, [RP2, Rdtb], [Rdt])
                ACT(dtv[:], dtv[:], AF.Exp, [Rdt], [Rdt])
                ACT(dtv[:], dtv[:], AF.Ln, [Rdt, Ronec], [Rdt], bias=onec[:, 0:1])
                TT(la[:], dtv[:], a_bc[:], ALU.mult, [Rdt, Ra_bc], [Rdt])
                for j in range(9):
                    u = c * NU + j
                    load_unit(u + 2)
                    sl = u % 3
                    w3 = wblk[sl].rearrange("p (a b) -> p a b", b=512)
                    i = pj[0] % 2
                    pj[0] += 1
                    ps, Rp = pf[i], Rpf[i]
                    if j < 3:
                        for kc in range(KC):
                            MM(ps[:, :], hc[:, kc, :], w3[:, kc, :], kc == 0, kc == KC - 1, [Rhc, Rwblk[sl]], [Rp])
                        ACT(sz[:, j * 512:(j + 1) * 512], ps[:, :], AF.Silu, [Rp], [Rsz])
                    elif j < 8:
                        jb = j - 3
                        for q in range(4):
                            for kc in range(KC):
                                MM(ps[:, q * 128:(q + 1) * 128], w3[:, kc, q * 128:(q + 1) * 128], hc[:, kc, :],
                                   kc == 0, kc == KC - 1, [Rhc, Rwblk[sl]], [Rp], inc=(kc == KC - 1 and q == 3))
                        pi = jb % 2
                        CP(pre[pi][:, :, 0:3], halo[:, jb * 4:(jb + 1) * 4, :], [Rhalo[jb]], [Rpre[pi]], eng="pool")
                        ACT(pre[pi][:, :, 3:131], ps[:].rearrange("p (a b) -> p a b", b=128), AF.Copy, [Rp], [Rpre[pi]])
                        CP(halo[:, jb * 4:(jb + 1) * 4, :], pre[pi][:, :, 128:131], [Rpre[pi]], [Rhalo[jb]], eng="pool")
                        for q in range(4):
                            cc = jb * 4 + q
                            k2 = tc[0] % 2
                            tc[0] += 1
                            ceng = "dve"
                            TS(cacc[k2][:], pre[pi][:, q, 0:128], convw[:, cc * 4:cc * 4 + 1], None, ALU.mult,
                               reads=[Rpre[pi], Rconvw], writes=[Rcacc[k2]], eng=ceng)
                            for k in range(1, 4):
                                STT(cacc[k2][:], pre[pi][:, q, k:k + 128], convw[:, cc * 4 + k:cc * 4 + k + 1], cacc[k2][:],
                                    ALU.mult, ALU.add, [Rpre[pi], Rconvw, Rcacc[k2]], [Rcacc[k2]], eng=ceng)
                            if cc < 12:
                                ACT(csf[k2][:], cacc[k2][:], AF.Silu, [Rcacc[k2], Rconvb], [Rcsf[k2]], bias=convb[:, cc:cc + 1])
                                P3, RP3 = pf[3], Rpf[3]
                                TR(P3[:, q * 128:(q + 1) * 128], csf[k2][:], identf[:], [Rcsf[k2], Ridentf], [RP3])
                                if q == 3:
                                    CP(xs_tok[:, jb * 512:(jb + 1) * 512], P3[:, :], [RP3], [Rxs])
                            elif cc < 16:
                                g = cc - 12
                                ACT(BT[:, g, :], cacc[k2][:], AF.Silu, [Rcacc[k2], Rconvb], [RBT], bias=convb[:, cc:cc + 1])
                                TR(pb[0][:, g * 128:(g + 1) * 128], BT[:, g, :], identb[:], [RBT, Rident], [Rpb[0]])
                                if g == 3:
                                    CP(Btok[:], pb[0][:, 0:512], [Rpb[0]], [RBtok])
                            else:
                                g = cc - 16
                                ACT(CT[:, g, :], cacc[k2][:], AF.Silu, [Rcacc[k2], Rconvb], [RCT], bias=convb[:, cc:cc + 1])
                    else:
                        for hm in range(4):
                            for kc in range(KC):
                                MM(ps[:, hm * 128:(hm + 1) * 128], w3[:, kc, hm * 128:(hm + 1) * 128], hc[:, kc, :],
                                   kc == 0, kc == KC - 1, [Rhc, Rwblk[sl]], [Rp], inc=(kc == KC - 1 and hm == 3))
                        ACT(qmT[:], ps[:].rearrange("p (a b) -> p a b", b=128), AF.Copy, [Rp], [RqmT], scale=SCALE)
                MM(P2[:, 0:24], U_sb[:], la[:], True, True, [RU, Rdt], [RP2], inc=False)
                MM(P2[:, 24:48], onesf[:], la[:], True, True, [Ronesf, Rdt], [RP2], inc=True)
                CP(lacs[:], P2[:, 0:24], [RP2], [Rdt])
                CP(tot[:], P2[:, 24:48], [RP2], [Rdt])
                ACT(expcs[:], lacs[:], AF.Exp, [Rdt], [Rdt])
                TT(dte[:], tot[:], lacs[:], ALU.subtract, [Rdt], [Rdt])
                ACT(dte[:], dte[:], AF.Exp, [Rdt], [Rdt])
                ACT(cdec[:], tot[:], AF.Exp, [Rdt], [Rdt])
                if c + 1 < NT:
                    cn = c + 1
                    norm_tile(x_sb[:, cn, :], Rx[cn], 1, hTc[cn % 2], RhTc[cn % 2], ss[:, cn:cn + 1], rstd[:, cn:cn + 1],
                              Rss[cn], Rrstd[cn])
                for hm in range(4):
                    attention_head(qmT[:, hm, :], RqmT,
                                   lambda kt, hm=hm: kTm[:, hm, kt * 128:(kt + 1) * 128], RkTm,
                                   lambda kt, hm=hm: vm[:, kt, hm * 128:(hm + 1) * 128], Rvm, 128,
                                   lambda a, b_, hm=hm: ycT[:, 12 + hm, a:b_], RycT[12 + hm], pTs, RpTs, rl, Rrl, moba=None)
                v3 = lambda a: a.rearrange("p (a b) -> p a b", b=64)
                b3 = lambda a, n: a.unsqueeze(2).to_broadcast([128, n, 64])
                TT(v3(xdt[:]), v3(xs_tok[:]), b3(dtv[:], 24), ALU.mult, [Rxs, Rdt], [Rxdt])
                TT(dte[:], dte[:], dtv[:], ALU.mult, [Rdt], [Rdt])
                TT(v3(xdte[:]), v3(xs_tok[:]), b3(dte[:], 24), ALU.mult, [Rxs, Rdt], [Rxdte])
                for g in range(4):
                    gi = g % 2
                    MM(P2[:, 128:256], BT[:, g, :], CT[:, g, :], True, True, [RBT, RCT], [RP2])
                    TT(CBm[gi][:], P2[:, 128:256], U_sb[:], ALU.mult, [RP2, RU], [RCBm[gi]])
                    for part, (h0, nh) in enumerate(((0, 4), (4, 2))):
                        bi = part
                        SEG, RSEG = pf[3 + bi], Rpf[3 + bi]
                        for hh in range(nh):
                            h = 6 * g + h0 + hh
                            li = (h0 + hh) % 4
                            TS(lh[li][:], SL_sb[:], la[:, h:h + 1], None, ALU.mult, reads=[RSL, Rdt], writes=[Rlh[li]], eng="pool")
                            MM(SEG[:, hh * 128:(hh + 1) * 128], lh[li][:], U_sb[:], True, True, [Rlh[li], RU], [RSEG],
                               inc=(hh == nh - 1))
                        ACT(dec[bi][:, 0:nh * 128], SEG[:, 0:nh * 128], AF.Exp, [RSEG], [Rdec[bi]])
                        TT(scT[bi][:, 0:nh, :], dec[bi][:, 0:nh * 128].rearrange("p (a b) -> p a b", b=128),
                           CBm[gi][:].unsqueeze(1).to_broadcast([128, nh, 128]), ALU.mult, [Rdec[bi], RCBm[gi]], [RscT[bi]])
                        for hh in range(nh):
                            h = 6 * g + h0 + hh
                            hl = h0 + hh
                            MM(pf[5][:, hl * 64:(hl + 1) * 64], scT[bi][:, hh, :], xdt[:, h * 64:(h + 1) * 64], True, True,
                               [RscT[bi], Rxdt], [Rpf[5]], inc=(hl == 5))
                    MM(pf[0][:, 0:384], CT[:, g, :], hbf[:, g, :], True, True, [RCT, Rhbf[g]], [Rpf[0]])
                    MM(pf[1][:, 0:384], Btok[:, g * 128:(g + 1) * 128], xdte[:, g * 384:(g + 1) * 384], True, True,
                       [RBtok, Rxdte], [Rpf[1]])
                    TT(v3(yoff[:]), v3(pf[0][:, 0:384]), b3(expcs[:, 6 * g:6 * g + 6], 6), ALU.mult, [Rpf[0], Rdt], [Ryoff])
                    TT(y_sb[:, g * 384:(g + 1) * 384], pf[5][:, 0:384], yoff[:], ALU.add, [Rpf[5], Ryoff], [Ry])
                    TT(v3(hst[:, g, :]), v3(hst[:, g, :]), b3(cdec[:, 6 * g:6 * g + 6], 6), ALU.mult, [Rhst[g], Rdt], [Rhst[g]])
                    TT(hst[:, g, :], hst[:, g, :], pf[1][:, 0:384], ALU.add, [Rhst[g], Rpf[1]], [Rhst[g]])
                    CP(hbf[:, g, :], hst[:, g, :], [Rhst[g]], [Rhbf[g]], eng="pool")
                TT(v3(ytmp[:]), v3(xs_tok[:]), b3(dskip[:], 24), ALU.mult, [Rxs, Rdskip], [Rytmp])
                TT(y_sb[:], y_sb[:], ytmp[:], ALU.add, [Ry, Rytmp], [Ry])
                TT(y_sb[:], y_sb[:], sz[:], ALU.mult, [Ry, Rsz], [Ry])
                MEMSET(gss[:], 0.0, [Rgss])
                for g in range(4):
                    ACT(junk[:, 0:384], y_sb[:, g * 384:(g + 1) * 384], AF.Square, [Ry, Rgss], [Rgss], accum_out=gss[:, g:g + 1])
                ACT(gss[:], gss[:], AF.Sqrt, [Rgss, Reps], [Rgss], bias=epsc[:, 0:1], scale=1.0 / 384)
                RECIP(gss[:], gss[:], [Rgss], [Rgss])
                g4 = lambda a: a.rearrange("p (a b) -> p a b", b=384)
                TT(g4(ytmp[:]), g4(y_sb[:]), gss[:].unsqueeze(2).to_broadcast([128, 4, 384]), ALU.mult, [Ry, Rgss], [Rytmp])
                TT(un[:], ytmp[:], gssd[:], ALU.mult, [Rytmp, Rgssd], [Run])
                for cc in range(12):
                    bi = 0 if cc < 8 else 1
                    off = cc if cc < 8 else cc - 8
                    TR(pb[bi][:, off * 128:(off + 1) * 128], un[:, cc * 128:(cc + 1) * 128], identb[:], [Run, Rident], [Rpb[bi]],
                       inc=(cc in (7, 11)))
                CP(ycT[:, 0:8, :], pb[0][:].rearrange("p (a b) -> p a b", b=128), [Rpb[0]], RycT[0:8])
                ACT(ycT[:, 8:12, :], pb[1][:, 0:512].rearrange("p (a b) -> p a b", b=128), AF.Copy, [Rpb[1]], RycT[8:12])
                for jb in range(4):
                    u = c * NU + 9 + jb
                    load_unit(u + 2)
                    sl = u % 3
                    w3 = wblk[sl].rearrange("p (a b) -> p a b", b=1024)
                    for q in range(4):
                        cidx = jb * 4 + q
                        for half in range(2):
                            MM(pf[half][:, :], ycT[:, cidx, :], w3[:, q, half * 512:(half + 1) * 512],
                               cidx == 0, cidx == 15, [RycT[cidx], Rwblk[sl]], [Rpf[half]],
                               inc=(q == 3 and half == 1))
                for half in range(2):
                    xs_ = x_sb[:, c, half * 512:(half + 1) * 512]
                    TT(xs_, xs_, pf[half][:, :], ALU.add, [Rpf[half], Rx[c]], [Rx[c]])
            em.barrier()

        def router():
            cv = Carver()
            lg = cv.take([128, 128], F32)
            lg2 = cv.take([128, 128], F32)
            eq = cv.take([128, 128], F32)
            mx1 = cv.take([128, 16], F32)
            mx2 = cv.take([128, 16], F32)
            Rl = R()
            G, RG = pf[0], Rpf[0]
            for t in range(NT):
                for kc in range(KC):
                    MM(G[:, t * 8:(t + 1) * 8], hT[:, kc, t * 128:(t + 1) * 128], wr_sb[:, kc * 8:(kc + 1) * 8],
                       kc == 0, kc == KC - 1, [RhT[t], Rwr], [RG], inc=(kc == KC - 1 and t == NT - 1))
            g3 = lambda a: a.rearrange("p (a b) -> p a b", b=8)
            bc = lambda a: a.unsqueeze(2).to_broadcast([128, 16, 8])
            TT(g3(lg[:]), g3(G[:, 0:128]), br_sb[:].unsqueeze(1).to_broadcast([128, 16, 8]), ALU.add, [RG, Rbr], [Rl])
            RED(mx1[:], g3(lg[:]), ALU.max, [Rl], [Rl])
            TT(g3(eq[:]), g3(lg[:]), bc(mx1[:]), ALU.is_equal, [Rl], [Rl])
            STT(lg2[:], eq[:], -BIG, lg[:], ALU.mult, ALU.add, [Rl], [Rl])
            RED(mx2[:], g3(lg2[:]), ALU.max, [Rl], [Rl])
            TT(g3(eq[:]), g3(lg[:]), bc(mx2[:]), ALU.is_ge, [Rl], [Rl])
            TT(g3(lg2[:]), g3(lg[:]), bc(mx1[:]), ALU.subtract, [Rl], [Rl])
            ACT(lg2[:], lg2[:], AF.Exp, [Rl], [Rl])
            TT(lg2[:], lg2[:], eq[:], ALU.mult, [Rl], [Rl])
            RED(mx1[:], g3(lg2[:]), ALU.add, [Rl], [Rl])
            RECIP(mx1[:], mx1[:], [Rl], [Rl])
            TT(g3(gw[:]), g3(lg2[:]), bc(mx1[:]), ALU.mult, [Rl], [Rgw])
            em.barrier()

        def layer1(s):
            mixer_ssd(s)
            if DBG == "nomoe":
                return
            norm_seq(3)
            router()
            nexp = 9 if not (DBG or "").startswith("moe") else 1 + int(DBG[3:])
            ffn_phase(list(range(1, nexp)), gw=gw[:].rearrange("p (a b) -> p a b", b=8), Rgw=Rgw)

        def final_norm(s):
            cv = Carver()
            ot = [cv.take([128, D], F32) for _ in range(2)]
            Rot = [R(), R()]
            for t in range(NT):
                i = t % 2
                MEMSET(ss[:, t:t + 1], 0.0, [Rss[t]])
                ACT(junk[:], x_sb[:, t, :], AF.Square, [Rx[t], Rss[t]], [Rss[t]], accum_out=ss[:, t:t + 1])
                ACT(rstd[:, t:t + 1], ss[:, t:t + 1], AF.Sqrt, [Rss[t], Reps], [Rrstd[t]], bias=epsc[:, 0:1], scale=1.0 / D)
                RECIP(rstd[:, t:t + 1], rstd[:, t:t + 1], [Rrstd[t]], [Rrstd[t]])
                STT(ot[i][:], x_sb[:, t, :], rstd[:, t:t + 1], gfin[:], ALU.mult, ALU.mult,
                    [Rx[t], Rrstd[t], Rgfin], [Rot[i]])
                em.dma("sp", "out", out_d[s, t * 128:(t + 1) * 128, :], ot[i][:], reads=[Rot[i]], writes=[Rout])
            em.barrier()

        Rout = R()

        wl1_bf = nc.dram_tensor("wl1_bf", [13, 128, 4096], BF16, kind="Internal").ap()
        Rwl1 = [R() for _ in range(13)]
        for j in range(13):
            src = wssd_d[j] if j < 9 else wout_d[1][:, (j - 9) * 4096:(j - 8) * 4096]
            em.dma("pool", "wl1cast%d" % (j % 4), wl1_bf[j], src, writes=[Rwl1[j]])

        for s in range(n_seq):
            for t4 in range(4):
                em.dma("sp", "xload%d" % t4, x_sb[:, t4 * 4:(t4 + 1) * 4, :],
                       x_d[s, t4 * 512:(t4 + 1) * 512, :].rearrange("(t p) d -> p t d", p=128),
                       writes=Rx[t4 * 4:(t4 + 1) * 4])
            norm_seq(0)
            mixer_moba(s)
            norm_seq(2)
            ffn_phase([0])
            if n_layers > 1:
                layer1(s)
            final_norm(s)

        em.finish()
    return nc


_SHARED_CACHE = {}


def _run(inputs, n_layers=2, n_seq=2, n_cores=8, core0=0):
    inp = {k: np.asarray(v) for k, v in inputs.items()}
    sh = _prep_shared(inp)
    nc = build(n_layers=n_layers, n_seq=n_seq)
    in_maps = []
    for c in range(core0, core0 + n_cores):
        m = dict(sh)
        m["x"] = np.ascontiguousarray(inp["x"][n_seq * c:n_seq * (c + 1)], dtype=np.float32)
        m["mem"] = np.ascontiguousarray(inp["mem"][n_seq * c:n_seq * (c + 1)], dtype=np.float32)
        in_maps.append(m)
    res = run_bass_kernel_spmd(nc, in_maps, core_ids=list(range(n_cores)))
    return np.concatenate([np.asarray(r["out"]) for r in res.results], axis=0)


N_CORES = 8
SEQ_PER_CORE = 16 // N_CORES


def kernel(**inputs):
    return _run(inputs, n_layers=2, n_seq=SEQ_PER_CORE, n_cores=N_CORES).astype(np.float32)
```

```python
import math
from contextlib import ExitStack
import numpy as np
import concourse.bass as bass
import concourse.mybir as mybir
from concourse.bass_utils import run_bass_kernel_spmd

F32 = mybir.dt.float32
BF16 = mybir.dt.bfloat16
AF = mybir.ActivationFunctionType
ALU = mybir.AluOpType
AX = mybir.AxisListType

S = 2048
D = 1024
NT = 16
KC = 8
EPS = 1e-6
SCALE = 128 ** -0.5
NEG = -30000.0
BIG = 1.0e30
DFF = 3584
NFB = 7
NEXP = 8
DBG = None
PACE_N = 512


class R:
    __slots__ = ("w", "r")

    def __init__(self):
        self.w = None
        self.r = []


class Emitter:
    ENG = ("pe", "act", "dve", "pool", "sp")

    def __init__(self, nc, stack):
        self.nc = nc
        self.stack = stack
        self.q = {e: [] for e in self.ENG}
        self.sems = {}
        self.cnt = {}
        self.waited = {}
        for e in self.ENG:
            self._mksem(e)
        self.n_instr = 0

    def _mksem(self, key):
        self.sems[key] = self.stack.enter_context(self.nc.semaphore("s_" + key))
        self.cnt[key] = 0

    def sbuf(self, name, shape, dt):
        return self.stack.enter_context(self.nc.sbuf_tensor("sb_" + name, list(shape), dt))

    def psum(self, name, shape, dt=F32):
        return self.stack.enter_context(self.nc.psum_tensor("ps_" + name, list(shape), dt))

    def _deps(self, eng, reads, writes):
        deps = {}

        def add(t):
            if t is None:
                return
            k, v = t
            if k == "pe" and eng == "pe":
                return
            if deps.get(k, 0) < v:
                deps[k] = v
        for r in reads:
            add(r.w)
        for w in writes:
            add(w.w)
            for t in w.r:
                add(t)
        out = []
        for k, v in deps.items():
            if self.waited.get((eng, k), 0) >= v:
                continue
            self.waited[(eng, k)] = v
            out.append((self.sems[k], v))
        return out

    def _mark(self, ticket, reads, writes):
        for r in reads:
            r.r.append(ticket)
            if len(r.r) > 48:
                best = {}
                for k, v in r.r:
                    if best.get(k, 0) < v:
                        best[k] = v
                r.r = list(best.items())
        for w in writes:
            w.w = ticket
            w.r = []

    def op(self, eng, fn, reads=(), writes=(), inc=True):
        waits = self._deps(eng, reads, writes)
        ticket = (eng, self.cnt[eng] + 1)
        if inc:
            self.cnt[eng] += 1
        sem = self.sems[eng]
        self._mark(ticket, reads, writes)
        self.n_instr += 1

        def thunk(e):
            for s, v in waits:
                e.wait_ge(s, v)
            ins = fn(e)
            if inc:
                ins.then_inc(sem, 1)
        self.q[eng].append(thunk)
        return ticket

    def dma(self, queue, slot, out, in_, reads=(), writes=()):
        key = "d_" + slot + "_" + queue
        if key not in self.sems:
            self._mksem(key)
        waits = self._deps(queue, reads, writes)
        self.cnt[key] += 16
        ticket = (key, self.cnt[key])
        sem = self.sems[key]
        self._mark(ticket, reads, writes)
        self.n_instr += 1

        def thunk(e):
            for s, v in waits:
                e.wait_ge(s, v)
            e.dma_start(out=out, in_=in_).then_inc(sem, 16)
        self.q[queue].append(thunk)
        return ticket

    def barrier(self):
        for eng in self.ENG:
            waits = []
            for k, v in self.cnt.items():
                if v == 0 or k == eng and eng in ("pe",):
                    continue
                if self.waited.get((eng, k), 0) >= v:
                    continue
                self.waited[(eng, k)] = v
                waits.append((self.sems[k], v))
            if waits:
                def thunk(e, waits=waits):
                    for s, v in waits:
                        e.wait_ge(s, v)
                self.q[eng].append(thunk)

    def finish(self):
        nc = self.nc
        q = self.q
        with nc.Block() as block:
            @block.tensor
            def _(e):
                for f in q["pe"]:
                    f(e)

            @block.scalar
            def _(e):
                for f in q["act"]:
                    f(e)

            @block.vector
            def _(e):
                for f in q["dve"]:
                    f(e)

            @block.gpsimd
            def _(e):
                for f in q["pool"]:
                    f(e)

            @block.sync
            def _(e):
                for f in q["sp"]:
                    f(e)


def _t5_bucket_np(dist):
    n = np.maximum(dist, 0).astype(np.int32)
    max_exact = 16
    nf = np.maximum(n, 1).astype(np.float32)
    large = max_exact + (np.log(nf / np.float32(max_exact)) / np.float32(math.log(128 / max_exact))
                         * np.float32(32 - max_exact)).astype(np.int32)
    large = np.minimum(large, 31)
    return np.where(n < max_exact, n, large)


def _consts():
    c = {}
    i = np.arange(128)
    c["identf"] = np.eye(128, dtype=np.float32)
    c["U"] = (i[:, None] <= i[None, :]).astype(np.float32)
    c["SL"] = (i[:, None] > i[None, :]).astype(np.float32)
    c["causneg"] = np.where(i[:, None] > i[None, :], NEG, 0.0).astype(np.float32)
    E = np.zeros((8, 8 * 128), np.float32)
    for b in range(8):
        E[b, b * 128:(b + 1) * 128] = 1.0
    c["E"] = E
    tb = np.arange(16)[:, None] // 2
    bb = np.arange(8)[None, :]
    past = (bb < tb).astype(np.float32).reshape(1, 128)
    own = (bb == tb).astype(np.float32).reshape(1, 128)
    c["past01"] = np.repeat(past, 128, 0)
    c["own01"] = np.repeat(own, 128, 0)
    c["pastneg"] = np.repeat(np.where(past > 0, 0.0, -BIG).astype(np.float32), 128, 0)
    return c


def _feat_major(v):
    n = v.shape[0] // 128
    return np.ascontiguousarray(v.reshape(n, 128).T)


def _bcast(v):
    return np.ascontiguousarray(np.broadcast_to(v.reshape(1, -1), (128, v.size)))


def _kmajor(w):
    k, n = w.shape
    return np.ascontiguousarray(w.reshape(k // 128, 128, n).transpose(1, 0, 2))


def _prep_shared(inp):
    f = np.float32
    sh = {}
    w0 = inp["w_in_moba"][0]
    wq = _kmajor(w0[:, 0:1536]).reshape(128, 8, 12, 128)
    wk = _kmajor(w0[:, 1536:3072]).reshape(128, 8, 12, 128)
    wv = _kmajor(w0[:, 3072:4608]).reshape(128, 8, 12, 128)
    wqkv = np.stack([wq, wk, wv], axis=3)
    sh["wqkv0"] = np.ascontiguousarray(wqkv.transpose(2, 0, 1, 3, 4)).reshape(12, 128, 8 * 384)
    wqm = _kmajor(w0[:, 4608:5120]).reshape(128, 8, 4, 128)
    sh["wqm0"] = np.ascontiguousarray(wqm.transpose(2, 0, 1, 3)).reshape(4, 128, 8 * 128)
    sh["wkv"] = np.stack([_kmajor(inp["w_mem_kv"][i]) for i in range(2)]).reshape(2, 128, 8 * 1024)
    sh["wout"] = np.stack([_kmajor(inp["w_out"][i]) for i in range(2)]).reshape(2, 128, 16 * 1024)
    wg = np.concatenate([inp["w_ffn_gate"], inp["w_exp_gate"][0]], 0)
    wu = np.concatenate([inp["w_ffn_up"], inp["w_exp_up"][0]], 0)
    wd = np.concatenate([inp["w_ffn_down"], inp["w_exp_down"][0]], 0)
    wgk = wg.reshape(9, 8, 128, NFB, 512).transpose(0, 3, 2, 1, 4)
    wuk = wu.reshape(9, 8, 128, NFB, 512).transpose(0, 3, 2, 1, 4)
    sh["wgu"] = np.ascontiguousarray(np.stack([wgk, wuk], axis=4)).reshape(9, NFB, 128, 8 * 2 * 512)
    sh["wd"] = np.ascontiguousarray(wd.reshape(9, NFB, 4, 128, 1024).transpose(0, 1, 3, 2, 4)).reshape(9, NFB, 128, 4 * 1024)
    w1 = inp["w_in_ssd"][0]
    blocks = [w1[:, j * 512:(j + 1) * 512] for j in range(8)] + [w1[:, 4120:4632]]
    sh["wssd"] = np.stack([_kmajor(b) for b in blocks]).reshape(9, 128, 8 * 512)
    sh["wdt"] = _kmajor(w1[:, 4096:4120]).reshape(128, 8 * 24)
    gv = np.stack([_feat_major(inp["g_mix"][0]), _feat_major(inp["g_mix"][1]),
                   _feat_major(inp["g_ffn"][0]), _feat_major(inp["g_ffn"][1]),
                   _feat_major(inp["g_mem"][0]), _feat_major(inp["g_mem"][1])], axis=1)
    sh["gvec"] = np.ascontiguousarray(gv).reshape(128, 48)
    sh["gfin"] = _bcast(inp["g_final"])
    cw = inp["conv_w"][0]
    sh["convw"] = np.ascontiguousarray(cw.reshape(4, 20, 128).transpose(2, 1, 0)).reshape(128, 80)
    sh["convb"] = _feat_major(inp["conv_b"][0])
    sh["dtb"] = _bcast(inp["dt_bias"][0])
    sh["alog"] = _bcast(inp["a_log"][0])
    sh["dskip"] = _bcast(inp["d_skip"][0])
    sh["gssd"] = _bcast(inp["g_ssd_out"][0])
    sh["wr"] = _kmajor(inp["w_router"][0]).reshape(128, 64)
    sh["br"] = _bcast(inp["b_router"][0])
    rb = inp["rel_bias"]
    i = np.arange(128)
    d0 = i[None, :] - i[:, None]
    bk0 = _t5_bucket_np(d0)
    bk1 = _t5_bucket_np(d0 + 128)
    tt = np.stack([rb[bk0], rb[bk1]], 0)
    sh["ttg"] = np.ascontiguousarray(tt.transpose(1, 0, 3, 2)).reshape(128, 2 * 12 * 128)
    sh["b31"] = _bcast(rb[31])
    sh.update(_consts())
    return {k: np.ascontiguousarray(v, dtype=f) for k, v in sh.items()}


def build(n_layers=2, n_seq=2):
    nc = bass.Bass("TRN2", target_bir_lowering=False)

    def din(name, shape):
        return nc.dram_tensor(name, list(shape), F32, kind="ExternalInput").ap()

    x_d = din("x", [n_seq, S, D])
    mem_d = din("mem", [n_seq, 256, D])
    wqkv0_d = din("wqkv0", [12, 128, 8 * 384])
    wqm0_d = din("wqm0", [4, 128, 8 * 128])
    wkv_d = din("wkv", [2, 128, 8 * 1024])
    wout_d = din("wout", [2, 128, 16 * 1024])
    wgu_d = din("wgu", [9, NFB, 128, 8 * 2 * 512])
    wd_d = din("wd", [9, NFB, 128, 4 * 1024])
    wssd_d = din("wssd", [9, 128, 8 * 512])
    wdt_d = din("wdt", [128, 8 * 24])
    gvec_d = din("gvec", [128, 48])
    gfin_d = din("gfin", [128, D])
    convw_d = din("convw", [128, 80])
    convb_d = din("convb", [128, 20])
    dtb_d = din("dtb", [128, 24])
    alog_d = din("alog", [128, 24])
    dskip_d = din("dskip", [128, 24])
    gssd_d = din("gssd", [128, 1536])
    wr_d = din("wr", [128, 64])
    br_d = din("br", [128, 8])
    ttg_d = din("ttg", [128, 2 * 12 * 128])
    b31_d = din("b31", [128, 12])
    identf_d = din("identf", [128, 128])
    U_d = din("U", [128, 128])
    SL_d = din("SL", [128, 128])
    causneg_d = din("causneg", [128, 128])
    E_d = din("E", [8, 1024])
    past01_d = din("past01", [128, 128])
    own01_d = din("own01", [128, 128])
    pastneg_d = din("pastneg", [128, 128])
    out_d = nc.dram_tensor("out", [n_seq, S, D], F32, kind="ExternalOutput").ap()

    with ExitStack() as st:
        em = Emitter(nc, st)

        def MM(out, lhsT, rhs, start, stop, reads, writes, inc=None):
            if inc is None:
                inc = stop
            em.op("pe", lambda e: e.matmul(out, lhsT=lhsT, rhs=rhs, start=start, stop=stop),
                  reads, writes, inc)

        def TR(out, in_, ident, reads, writes, inc=True):
            em.op("pe", lambda e: e.transpose(out=out, in_=in_, identity=ident), reads, writes, inc)

        def ACT(out, in_, func, reads, writes, bias=None, scale=None, accum_out=None):
            kw = {}
            if bias is not None:
                kw["bias"] = bias
            if scale is not None:
                kw["scale"] = scale
            if accum_out is not None:
                kw["accum_out"] = accum_out
            em.op("act", lambda e: e.activation(out=out, in_=in_, func=func, **kw), reads, writes)

        def TT(out, in0, in1, op, reads, writes, eng="dve"):
            em.op(eng, lambda e: e.tensor_tensor(out=out, in0=in0, in1=in1, op=op), reads, writes)

        def TS(out, in0, s1, s2, op0, op1=None, reads=(), writes=(), eng="dve", accum_out=None):
            kw = {}
            if op1 is not None:
                kw["op1"] = op1
            if accum_out is not None:
                kw["accum_out"] = accum_out
            em.op(eng, lambda e: e.tensor_scalar(out=out, in0=in0, scalar1=s1, scalar2=s2, op0=op0, **kw),
                  reads, writes)

        def STT(out, in0, scalar, in1, op0, op1, reads, writes, eng="dve"):
            em.op(eng, lambda e: e.scalar_tensor_tensor(out=out, in0=in0, scalar=scalar, in1=in1,
                                                        op0=op0, op1=op1), reads, writes)

        def CP(out, in_, reads, writes, eng="dve"):
            em.op(eng, lambda e: e.tensor_copy(out=out, in_=in_), reads, writes)

        def RED(out, in_, op, reads, writes, eng="dve"):
            em.op(eng, lambda e: e.tensor_reduce(out=out, in_=in_, axis=AX.X, op=op), reads, writes)

        def RECIP(out, in_, reads, writes):
            em.op("dve", lambda e: e.reciprocal(out=out, in_=in_), reads, writes)

        def MEMSET(ap, val, writes, eng="pool", reads=()):
            em.op(eng, lambda e: e.memset(ap, val), reads, writes)

        x_sb = em.sbuf("x_sb", [128, NT, D], F32)
        Rx = [R() for _ in range(NT)]
        RhT = [R() for _ in range(NT)]

        def cload(name, src, shape, dt=F32, queue=None):
            t = em.sbuf(name, shape, dt)
            r = R()
            q = queue or ("pool" if dt != F32 else "sp")
            flat = t[:] if len(shape) == 2 else t[:]
            em.dma(q, "const", flat, src, writes=[r])
            return t, r

        identb, Rident = cload("identb", identf_d, [128, 128], BF16)
        identf, Ridentf = cload("identf", identf_d, [128, 128])
        U_sb, RU = cload("U_sb", U_d, [128, 128])
        SL_sb, RSL = cload("SL_sb", SL_d, [128, 128])
        causneg, Rcaus = cload("causneg", causneg_d, [128, 128])
        E_sb, RE = cload("E_sb", E_d, [8, 1024], BF16)
        past01, Rpast = cload("past01", past01_d, [128, 128])
        own01, Rown = cload("own01", own01_d, [128, 128])
        pastneg, Rpastneg = cload("pastneg", pastneg_d, [128, 128])
        gvec, Rgvec = cload("gvec", gvec_d, [128, 48])
        gfin, Rgfin = cload("gfin", gfin_d, [128, D])
        b31, Rb31 = cload("b31", b31_d, [128, 12])
        onesb = em.sbuf("onesb", [128, 128], BF16)
        Rones = R()
        MEMSET(onesb[:], 1.0, [Rones])
        onesf = em.sbuf("onesf", [128, 128], F32)
        Ronesf = R()
        MEMSET(onesf[:], 1.0, [Ronesf])
        junk = em.sbuf("junk", [128, D], BF16)
        epsc = em.sbuf("epsc", [128, 1], F32)
        Reps = R()
        MEMSET(epsc[:], EPS, [Reps])
        ss = em.sbuf("ss", [128, NT], F32)
        rstd = em.sbuf("rstd", [128, NT], F32)
        Rss = [R() for _ in range(NT)]
        Rrstd = [R() for _ in range(NT)]
        xn = [em.sbuf("xn%d" % i, [128, D], BF16) for i in range(2)]
        Rxn = [R(), R()]

        wdt_sb, Rwdt = cload("wdt_sb", wdt_d, [128, 192], BF16)
        convw, Rconvw = cload("convw_sb", convw_d, [128, 80])
        convb, Rconvb = cload("convb_sb", convb_d, [128, 20])
        dtb, Rdtb = cload("dtb_sb", dtb_d, [128, 24])
        a_bc, Ra_bc = cload("a_bc", alog_d, [128, 24])
        dskip, Rdskip = cload("dskip_sb", dskip_d, [128, 24])
        wr_sb, Rwr = cload("wr_sb", wr_d, [128, 64], BF16)
        br_sb, Rbr = cload("br_sb", br_d, [128, 8])
        gw = em.sbuf("gw", [128, 128], F32)
        Rgw = R()
        onec = em.sbuf("onec", [128, 1], F32)
        Ronec = R()
        MEMSET(onec[:], 1.0, [Ronec])

        TTb = em.sbuf("TTb", [128, 2 * 12 * 128], BF16)
        RTTb = R()

        pf = [em.psum("pf%d" % i, [128, 512], F32) for i in range(6)]
        Rpf = [R() for _ in range(6)]
        pb = [em.psum("pb%d" % i, [128, 1024], BF16) for i in range(2)]
        Rpb = [R(), R()]

        ARENA = 86 * 1024
        ARENA_FULL = ARENA + 32 * 1024
        arena = em.sbuf("arena", [128, ARENA_FULL // 2], BF16)
        hT = arena[:, ARENA // 2: ARENA_FULL // 2].rearrange("p (a b) -> p a b", b=S)

        class Carver:
            def __init__(self, limit=None):
                self.off = 0
                self.limit = limit or ARENA

            def take(self, shape, dt, parts=128):
                esz = 4 if dt == F32 else 2
                n = int(np.prod(shape[1:]))
                nbytes = (n * esz + 31) // 32 * 32
                assert self.off + nbytes <= self.limit, ("arena overflow", self.off, nbytes)
                ap = arena[0:shape[0], self.off // 2: self.off // 2 + n * esz // 2]
                if dt == F32:
                    ap = ap.bitcast(F32)
                self.off += nbytes
                if len(shape) == 3:
                    ap = ap.rearrange("p (a b) -> p a b", b=shape[2])
                elif len(shape) == 4:
                    ap = ap.rearrange("p (a b c) -> p a b c", b=shape[2], c=shape[3])
                return ap

        def setup_bias():
            cv = Carver()
            ttg = cv.take([128, 2 * 12 * 128], F32)
            Rttg = R()
            em.dma("sp", "const", ttg, ttg_d, writes=[Rttg])
            em.barrier()
            ACT(a_bc[:], a_bc[:], AF.Exp, [Ra_bc], [Ra_bc])
            TS(a_bc[:], a_bc[:], -1.0, None, ALU.mult, reads=[Ra_bc], writes=[Ra_bc])
            for j in range(2):
                for h in range(12):
                    sl = slice((j * 12 + h) * 128, (j * 12 + h + 1) * 128)
                    if j == 0:
                        STT(TTb[:, sl], ttg[:, sl], b31[:, h:h + 1], causneg[:], ALU.subtract, ALU.add,
                            [Rttg, Rb31, Rcaus], [RTTb])
                    else:
                        TS(TTb[:, sl], ttg[:, sl], b31[:, h:h + 1], None, ALU.subtract,
                           reads=[Rttg, Rb31], writes=[RTTb])
            em.barrier()

        setup_bias()

        nrm_ctr = [0]

        def norm_tile(src_ap, Rsrc, gi, dst_ap, Rdst, ss_ap, rstd_ap, Rs, Rr, ncols=D):
            i = nrm_ctr[0] % 2
            nrm_ctr[0] += 1
            MEMSET(ss_ap, 0.0, [Rs])
            ACT(junk[:], src_ap, AF.Square, [Rsrc, Rs], [Rs], accum_out=ss_ap)
            ACT(rstd_ap, ss_ap, AF.Sqrt, [Rs, Reps], [Rr], bias=epsc[:, 0:1], scale=1.0 / D)
            RECIP(rstd_ap, rstd_ap, [Rr], [Rr])
            TS(xn[i][:], src_ap, rstd_ap, None, ALU.mult, reads=[Rsrc, Rr], writes=[Rxn[i]])
            for kc in range(KC):
                TR(pb[i][:, kc * 128:(kc + 1) * 128], xn[i][:, kc * 128:(kc + 1) * 128], identb[:],
                   [Rxn[i], Rident], [Rpb[i]], inc=(kc == KC - 1))
            gb = gvec[:, gi * 8:(gi + 1) * 8].unsqueeze(2).to_broadcast([128, 8, 128])
            TT(dst_ap, pb[i][:].rearrange("p (a b) -> p a b", b=128), gb, ALU.mult,
               [Rpb[i], Rgvec], [Rdst])

        def norm_seq(gi):
            for t in range(NT):
                norm_tile(x_sb[:, t, :], Rx[t], gi, hT[:, :, t * 128:(t + 1) * 128], RhT[t],
                          ss[:, t:t + 1], rstd[:, t:t + 1], Rss[t], Rrstd[t])

        pT_ctr = [0]
        s_ctr = [0]

        def attention_head(qT_ap, RqT, kT_fn, RkT, V_fn, RV, n_q, out_fn, Rout, pTs, RpTs, rl, Rrl,
                           moba=None):
            O, L = pf[4], pf[5]
            RO, RL = Rpf[4], Rpf[5]
            ngrp = (n_q + 511) // 512
            pending = None

            def emit_pv(kt, c0, qn, pi, first, last):
                MM(O[:, c0:qn], V_fn(kt), pTs[pi][:, c0:qn], first, last, [RV, RpTs[pi]], [RO], inc=False)
                MM(L[:, c0:qn], onesb[:], pTs[pi][:, c0:qn], first, last, [Rones, RpTs[pi]], [RL], inc=True)

            for qg in range(ngrp):
                q0 = qg * 512
                qn = min(512, n_q - q0)
                if moba is None:
                    kts = [0, 1]
                else:
                    kts = list(range(0, 4 * qg + 4))
                for idx, kt in enumerate(kts):
                    if moba is None:
                        c0 = 0
                    else:
                        c0 = max(0, kt - 4 * qg) * 128
                    si = s_ctr[0] % 2
                    s_ctr[0] += 1
                    Sb, RS = pf[2 + si], Rpf[2 + si]
                    extra = []
                    if moba is not None:
                        b = kt // 2
                        extra.append((E_sb[0:8, b * 128:(b + 1) * 128], moba["MnegT"][0:8, q0 + c0:q0 + qn],
                                      c0, qn, [RE, moba["RM"]]))
                        for j in range(2):
                            qt = kt + j
                            cc = (qt - 4 * qg) * 128
                            if 0 <= cc < qn and cc >= c0:
                                sl = slice((j * 12 + moba["h"]) * 128, (j * 12 + moba["h"] + 1) * 128)
                                extra.append((identb[:], TTb[:, sl], cc, cc + 128, [Rident, RTTb]))
                    MM(Sb[:, c0:qn], kT_fn(kt), qT_ap[:, q0 + c0:q0 + qn], True, len(extra) == 0,
                       [RkT, RqT], [RS])
                    for ei, (l_, r_, a0, a1, rr) in enumerate(extra):
                        MM(Sb[:, a0:a1], l_, r_, False, ei == len(extra) - 1, rr, [RS])
                    pi = pT_ctr[0] % len(pTs)
                    pT_ctr[0] += 1
                    ACT(pTs[pi][:, c0:qn], Sb[:, c0:qn], AF.Exp, [RS], [RpTs[pi]])
                    if pending is not None:
                        emit_pv(*pending)
                    pending = (kt, c0, qn, pi, idx == 0, idx == len(kts) - 1)
                emit_pv(*pending)
                pending = None
                RECIP(rl[:, 0:qn], L[:, 0:qn], [RL], [Rrl])
                TT(out_fn(q0, q0 + qn), O[:, 0:qn], rl[:, 0:qn], ALU.mult, [RO, Rrl], [Rout])

        def mem_kv(s, layer, cv, kTm, RkTm, vm, Rvm):
            memx = cv.take([128, 2, D], F32)
            Rmemx = [R(), R()]
            memnT = cv.take([128, KC, 256], BF16)
            RmemnT = [R(), R()]
            wkv = cv.take([128, KC, 1024], BF16)
            Rwkv = R()
            mss = cv.take([128, 2], F32)
            mrs = cv.take([128, 2], F32)
            Rm1 = [R(), R()]
            Rm2 = [R(), R()]
            em.dma("pool", "wkv", wkv.rearrange("p a b -> p (a b)"), wkv_d[layer], writes=[Rwkv])
            for mt in range(2):
                em.dma("sp", "memx%d" % mt, memx[:, mt, :], mem_d[s, mt * 128:(mt + 1) * 128, :], writes=[Rmemx[mt]])
            for mt in range(2):
                norm_tile(memx[:, mt, :], Rmemx[mt], 4 + layer, memnT[:, :, mt * 128:(mt + 1) * 128], RmemnT[mt],
                          mss[:, mt:mt + 1], mrs[:, mt:mt + 1], Rm1[mt], Rm2[mt])
            for hm in range(4):
                ps, Rp = pf[hm % 2], Rpf[hm % 2]
                for kc in range(KC):
                    MM(ps[:, 0:256], wkv[:, kc, hm * 128:(hm + 1) * 128], memnT[:, kc, :], kc == 0, kc == KC - 1,
                       [Rwkv] + RmemnT, [Rp])
                CP(kTm[:, hm, :], ps[:, 0:256], [Rp], [RkTm])
            for mt in range(2):
                ps, Rp = pf[mt % 2], Rpf[mt % 2]
                for kc in range(KC):
                    MM(ps[:, :], memnT[:, kc, mt * 128:(mt + 1) * 128], wkv[:, kc, 512:1024], kc == 0, kc == KC - 1,
                       [Rwkv, RmemnT[mt]], [Rp])
                ACT(vm[:, mt, :], ps[:, :], AF.Copy, [Rp], [Rvm])

        def mixer_moba(s):
            cv = Carver()
            kTm = cv.take([128, 4, 256], BF16)
            RkTm = R()
            vm = cv.take([128, 2, 512], BF16)
            Rvm = R()
            mark = cv.off
            mem_kv(s, 0, cv, kTm, RkTm, vm, Rvm)
            em.barrier()
            cv.off = mark
            wq = [cv.take([128, KC, 384], BF16) for _ in range(2)]
            Rwq = [R(), R()]
            qT = [cv.take([128, S], BF16) for _ in range(2)]
            RqT = [R(), R()]
            kT = [cv.take([128, S], BF16) for _ in range(2)]
            RkT = [R(), R()]
            Vh = [cv.take([128, NT, 128], BF16) for _ in range(2)]
            RV = [R(), R()]
            oTg = cv.take([128, 4, S], BF16)
            RoTg = [R() for _ in range(4)]
            woutg = cv.take([128, 4, D], BF16)
            Rwoutg = R()
            pTs = [cv.take([128, 512], BF16) for _ in range(3)]
            RpTs = [R() for _ in range(3)]
            rl = cv.take([128, 512], F32)
            Rrl = R()
            gm = cv.take([128, 128], F32)
            gm2 = cv.take([128, 128], F32)
            eq = cv.take([128, 128], F32)
            mx = cv.take([128, 16], F32)
            Rg = R()
            Mneg = cv.take([128, 128], BF16)
            RMneg = R()
            MnegT = cv.take([8, S], BF16, parts=8)
            RMnegT = R()
            km = cv.take([128, 8], F32)
            kmb = cv.take([128, 8], BF16)
            Rkm = R()

            def load_w(h):
                sl = h % 2
                if h < 12:
                    em.dma("pool", "wq%d" % sl, wq[sl].rearrange("p a b -> p (a b)"), wqkv0_d[h], writes=[Rwq[sl]])
                else:
                    em.dma("pool", "wq%d" % sl, wq[sl][:, :, 0:128], wqm0_d[h - 12].rearrange("p (a b) -> p a b", b=128),
                           writes=[Rwq[sl]])

            load_w(0)
            pj = [0]

            def proj_fm(dst, Rdst, w, Rw, c0, scale, use_act):
                for tg in range(4):
                    i = pj[0] % 2
                    pj[0] += 1
                    ps, Rp = pf[i], Rpf[i]
                    for kc in range(KC):
                        MM(ps[:, :], w[:, kc, c0:c0 + 128], hT[:, kc, tg * 512:(tg + 1) * 512], kc == 0, kc == KC - 1,
                           [Rw] + RhT[tg * 4:tg * 4 + 4], [Rp])
                    if use_act:
                        ACT(dst[:, tg * 512:(tg + 1) * 512], ps[:, :], AF.Copy, [Rp], [Rdst], scale=scale)
                    else:
                        CP(dst[:, tg * 512:(tg + 1) * 512], ps[:, :], [Rp], [Rdst])

            for h in range(16):
                sl = h % 2
                grp, hh = h // 4, h % 4
                if h + 1 < 16:
                    load_w(h + 1)
                if hh == 0:
                    em.dma("pool", "woutg", woutg.rearrange("p a b -> p (a b)"),
                           wout_d[0][:, grp * 4096:(grp + 1) * 4096], writes=[Rwoutg])
                proj_fm(qT[sl], RqT[sl], wq[sl], Rwq[sl], 0, SCALE, True)
                if h < 12:
                    proj_fm(kT[sl], RkT[sl], wq[sl], Rwq[sl], 128, None, False)
                    RED(km[:], kT[sl].rearrange("p (a b) -> p a b", b=256), ALU.add, [RkT[sl]], [Rkm])
                    TS(kmb[:], km[:], 1.0 / 256, None, ALU.mult, reads=[Rkm], writes=[Rkm])
                    G, RG = pf[0], Rpf[0]
                    pj[0] = 1
                    for t in range(NT):
                        MM(G[:, t * 8:(t + 1) * 8], qT[sl][:, t * 128:(t + 1) * 128], kmb[:], True, True,
                           [RqT[sl], Rkm], [RG], inc=(t == NT - 1))
                    g3 = lambda a: a.rearrange("p (a b) -> p a b", b=8)
                    mb = lambda: mx[:].unsqueeze(2).to_broadcast([128, 16, 8])
                    TT(gm[:], G[:, 0:128], pastneg[:], ALU.add, [RG, Rpastneg], [Rg])
                    for t4 in range(4):
                        i = pj[0] % 2
                        pj[0] += 1
                        ps, Rp = pf[i], Rpf[i]
                        for j in range(4):
                            t = t4 * 4 + j
                            for kc in range(KC):
                                MM(ps[:, j * 128:(j + 1) * 128], hT[:, kc, t * 128:(t + 1) * 128], wq[sl][:, kc, 256:384],
                                   kc == 0, kc == KC - 1, [Rwq[sl], RhT[t]], [Rp], inc=(kc == KC - 1 and j == 3))
                        ACT(Vh[sl][:, t4 * 4:(t4 + 1) * 4, :], ps[:].rearrange("p (a b) -> p a b", b=128), AF.Copy,
                            [Rp], [RV[sl]])
                    RED(mx[:], g3(gm[:]), ALU.max, [Rg], [Rg])
                    TT(g3(eq[:]), g3(gm[:]), mb(), ALU.is_equal, [Rg], [Rg])
                    STT(gm2[:], eq[:], -BIG, gm[:], ALU.mult, ALU.add, [Rg], [Rg])
                    RED(mx[:], g3(gm2[:]), ALU.max, [Rg], [Rg])
                    TT(g3(eq[:]), g3(gm2[:]), mb(), ALU.is_equal, [Rg], [Rg])
                    STT(gm2[:], eq[:], -BIG, gm2[:], ALU.mult, ALU.add, [Rg], [Rg])
                    RED(mx[:], g3(gm2[:]), ALU.max, [Rg], [Rg])
                    TT(g3(eq[:]), g3(gm[:]), mb(), ALU.is_ge, [Rg], [Rg])
                    TT(eq[:], eq[:], past01[:], ALU.mult, [Rg, Rpast], [Rg])
                    TT(eq[:], eq[:], own01[:], ALU.add, [Rg, Rown], [Rg])
                    TS(Mneg[:], eq[:], -1.0, -NEG, ALU.add, ALU.mult, reads=[Rg], writes=[RMneg])
                    for half in range(2):
                        pbi = half
                        for t8 in range(8):
                            t = half * 8 + t8
                            TR(pb[pbi][0:8, t8 * 128:(t8 + 1) * 128], Mneg[:, t * 8:(t + 1) * 8], identb[:],
                               [RMneg, Rident], [Rpb[pbi]], inc=(t8 == 7))
                        ACT(MnegT[0:8, half * 1024:(half + 1) * 1024], pb[pbi][0:8, :], AF.Copy, [Rpb[pbi]], [RMnegT])
                    attention_head(qT[sl], RqT[sl],
                                   lambda kt, sl=sl: kT[sl][:, kt * 128:(kt + 1) * 128], RkT[sl],
                                   lambda kt, sl=sl: Vh[sl][:, kt, :], RV[sl], S,
                                   lambda a, b_, hh=hh: oTg[:, hh, a:b_], RoTg[hh], pTs, RpTs, rl, Rrl,
                                   moba=dict(h=h, MnegT=MnegT, RM=RMnegT))
                else:
                    hm = h - 12
                    attention_head(qT[sl], RqT[sl],
                                   lambda kt, hm=hm: kTm[:, hm, kt * 128:(kt + 1) * 128], RkTm,
                                   lambda kt, hm=hm: vm[:, kt, hm * 128:(hm + 1) * 128], Rvm, S,
                                   lambda a, b_, hh=hh: oTg[:, hh, a:b_], RoTg[hh], pTs, RpTs, rl, Rrl, moba=None)
                if hh == 3:
                    for t in range(NT):
                        for half in range(2):
                            i = pj[0] % 2
                            pj[0] += 1
                            ps, Rp = pf[i], Rpf[i]
                            for c in range(4):
                                MM(ps[:, :], oTg[:, c, t * 128:(t + 1) * 128], woutg[:, c, half * 512:(half + 1) * 512],
                                   c == 0, c == 3, [RoTg[c], Rwoutg], [Rp])
                            TT(x_sb[:, t, half * 512:(half + 1) * 512], x_sb[:, t, half * 512:(half + 1) * 512], ps[:, :],
                               ALU.add, [Rp, Rx[t]], [Rx[t]])
            em.barrier()

        ffn_state = {"n": 0}

        def ffn_phase(experts, gw=None, Rgw=None):
            cv = Carver()
            wgu = [cv.take([128, KC, 2, 512], BF16) for _ in range(2)]
            wdn = [cv.take([128, 4, D], BF16) for _ in range(2)]
            Rw = [R(), R()]
            act = cv.take([128, 4, S], BF16)
            Ract = [R() for _ in range(4)]
            sg = [cv.take([128, 512], F32) for _ in range(2)]
            Rsg = [R(), R()]
            units = [(e, fb) for e in experts for fb in range(NFB)]
            pace_buf = cv.take([128, max(PACE_N, 8)], F32)
            Rpace = R()

            def load(u):
                e, fb = units[u]
                sl = u % 2
                em.dma("pool", "wgu%d" % sl, wgu[sl].rearrange("p a b c -> p (a b c)"), wgu_d[e, fb], writes=[Rw[sl]])
                em.dma("pool", "wgu%d" % sl, wdn[sl].rearrange("p a b -> p (a b)"), wd_d[e, fb], writes=[Rw[sl]])

            load(0)
            ctr = 0
            for u, (e, fb) in enumerate(units):
                sl = u % 2
                if u + 1 < len(units):
                    load(u + 1)
                for tg in range(4):
                    for fc in range(4):
                        i = ctr % 2
                        ctr += 1
                        gp, Rgp = pf[i], Rpf[i]
                        up, Rup = pf[2 + i], Rpf[2 + i]
                        rds = [Rw[sl]] + RhT[tg * 4:tg * 4 + 4] + ([Rpace] if PACE_N else [])
                        for kc in range(KC):
                            MM(gp[:, :], wgu[sl][:, kc, 0, fc * 128:(fc + 1) * 128], hT[:, kc, tg * 512:(tg + 1) * 512],
                               kc == 0, kc == KC - 1, rds, [Rgp])
                        for kc in range(KC):
                            MM(up[:, :], wgu[sl][:, kc, 1, fc * 128:(fc + 1) * 128], hT[:, kc, tg * 512:(tg + 1) * 512],
                               kc == 0, kc == KC - 1, rds, [Rup])
                        if PACE_N:
                            MEMSET(pace_buf[:, 0:PACE_N], 0.0, [Rpace], reads=[Rgp, Rup])
                        ACT(sg[i][:], gp[:, :], AF.Silu, [Rgp], [Rsg[i]])
                        TT(act[:, fc, tg * 512:(tg + 1) * 512], sg[i][:], up[:, :], ALU.mult, [Rsg[i], Rup], [Ract[fc]])
                for t in range(NT):
                    for half in range(2):
                        i = ctr % 2
                        ctr += 1
                        yp, Ryp = pf[4 + i], Rpf[4 + i]
                        pace_here = PACE_N and (t * 2 + half) % 4 == 0
                        for fc in range(4):
                            MM(yp[:, :], act[:, fc, t * 128:(t + 1) * 128], wdn[sl][:, fc, half * 512:(half + 1) * 512],
                               fc == 0, fc == 3, [Ract[fc], Rw[sl]] + ([Rpace] if pace_here else []), [Ryp])
                        if PACE_N and (t * 2 + half) % 4 == 3:
                            MEMSET(pace_buf[:, 0:PACE_N], 0.0, [Rpace], reads=[Ryp])
                        xs_ = x_sb[:, t, half * 512:(half + 1) * 512]
                        if gw is None:
                            TT(xs_, xs_, yp[:, :], ALU.add, [Ryp, Rx[t]], [Rx[t]])
                        else:
                            STT(xs_, yp[:, :], gw[:, t, e - 1:e], xs_, ALU.mult, ALU.add, [Ryp, Rx[t], Rgw], [Rx[t]])
            em.barrier()

        def mixer_ssd(s):
            cv = Carver(ARENA_FULL)
            kTm = cv.take([128, 4, 256], BF16)
            RkTm = R()
            vm = cv.take([128, 2, 512], BF16)
            Rvm = R()
            mark = cv.off
            mem_kv(s, 1, cv, kTm, RkTm, vm, Rvm)
            em.barrier()
            cv.off = mark
            wblk = [cv.take([128, 4096], BF16) for _ in range(3)]
            Rwblk = [R() for _ in range(3)]
            hTc = [cv.take([128, KC, 128], BF16) for _ in range(2)]
            RhTc = [R(), R()]
            sz = cv.take([128, 1536], F32)
            Rsz = R()
            pre = [cv.take([128, 4, 131], F32) for _ in range(2)]
            Rpre = [R(), R()]
            halo = cv.take([128, 20, 3], F32)
            Rhalo = [R() for _ in range(5)]
            cacc = [cv.take([128, 128], F32) for _ in range(2)]
            Rcacc = [R(), R()]
            csf = [cv.take([128, 128], F32) for _ in range(2)]
            Rcsf = [R(), R()]
            xs_tok = cv.take([128, 1536], F32)
            Rxs = R()
            BT = cv.take([128, 4, 128], BF16)
            RBT = R()
            CT = cv.take([128, 4, 128], BF16)
            RCT = R()
            Btok = cv.take([128, 512], BF16)
            RBtok = R()
            dtv = cv.take([128, 24], F32)
            la = cv.take([128, 24], F32)
            lacs = cv.take([128, 24], F32)
            tot = cv.take([128, 24], F32)
            expcs = cv.take([128, 24], F32)
            dte = cv.take([128, 24], F32)
            cdec = cv.take([128, 24], F32)
            Rdt = R()
            xdt = cv.take([128, 1536], BF16)
            Rxdt = R()
            xdte = cv.take([128, 1536], BF16)
            Rxdte = R()
            lh = [cv.take([128, 128], F32) for _ in range(8)]
            Rlh = [R() for _ in range(8)]
            dec = [cv.take([128, 512], F32) for _ in range(2)]
            Rdec = [R(), R()]
            scT = [cv.take([128, 4, 128], BF16) for _ in range(2)]
            RscT = [R(), R()]
            CBm = [cv.take([128, 128], F32) for _ in range(2)]
            RCBm = [R(), R()]
            hst = cv.take([128, 4, 384], F32)
            hbf = cv.take([128, 4, 384], BF16)
            Rhst = [R() for _ in range(4)]
            Rhbf = [R() for _ in range(4)]
            y_sb = cv.take([128, 1536], F32)
            Ry = R()
            ytmp = cv.take([128, 1536], F32)
            Rytmp = R()
            yoff = cv.take([128, 384], F32)
            Ryoff = R()
            gss = cv.take([128, 4], F32)
            Rgss = R()
            un = cv.take([128, 1536], BF16)
            Run = R()
            ycT = cv.take([128, 16, 128], BF16)
            RycT = [R() for _ in range(16)]
            qmT = cv.take([128, 4, 128], BF16)
            RqmT = R()
            pTs = [cv.take([128, 512], BF16) for _ in range(3)]
            RpTs = [R() for _ in range(3)]
            rl = cv.take([128, 512], F32)
            Rrl = R()
            gssd = cv.take([128, 1536], F32)
            Rgssd = R()
            em.dma("sp", "gssd", gssd, gssd_d, writes=[Rgssd])
            MEMSET(halo.rearrange("p a b -> p (a b)"), 0.0, Rhalo)
            MEMSET(hst.rearrange("p a b -> p (a b)"), 0.0, Rhst)
            MEMSET(hbf.rearrange("p a b -> p (a b)"), 0.0, Rhbf)

            NU = 13

            def load_unit(u):
                c, j = divmod(u, NU)
                if c >= NT:
                    return
                sl = u % 3
                em.dma("sp", "wblk%d" % sl, wblk[sl], wl1_bf[j], reads=[Rwl1[j]], writes=[Rwblk[sl]])

            load_unit(0)
            load_unit(1)
            pj = [0]
            tc = [0]
            norm_tile(x_sb[:, 0, :], Rx[0], 1, hTc[0], RhTc[0], ss[:, 0:1], rstd[:, 0:1], Rss[0], Rrstd[0])
            for c in range(NT):
                ci = c % 2
                hc = hTc[ci]
                Rhc = RhTc[ci]
                P2, RP2 = pf[2], Rpf[2]
                for kc in range(KC):
                    MM(P2[:, 0:24], hc[:, kc, :], wdt_sb[:, kc * 24:(kc + 1) * 24], kc == 0, kc == KC - 1, [Rhc, Rwdt], [RP2])
                TT(dtv[:], P2[:, 0:24], dtb[:], ALU.add, [RP2, Rdtb], [Rdt])
                ACT(dtv[:], dtv[:], AF.Exp, [Rdt], [Rdt])
                ACT(dtv[:], dtv[:], AF.Ln, [Rdt, Ronec], [Rdt], bias=onec[:, 0:1])
                TT(la[:], dtv[:], a_bc[:], ALU.mult, [Rdt, Ra_bc], [Rdt])
                for j in range(9):
                    u = c * NU + j
                    load_unit(u + 2)
                    sl = u % 3
                    w3 = wblk[sl].rearrange("p (a b) -> p a b", b=512)
                    i = pj[0] % 2
                    pj[0] += 1
                    ps, Rp = pf[i], Rpf[i]
                    if j < 3:
                        for kc in range(KC):
                            MM(ps[:, :], hc[:, kc, :], w3[:, kc, :], kc == 0, kc == KC - 1, [Rhc, Rwblk[sl]], [Rp])
                        ACT(sz[:, j * 512:(j + 1) * 512], ps[:, :], AF.Silu, [Rp], [Rsz])
                    elif j < 8:
                        jb = j - 3
                        for q in range(4):
                            for kc in range(KC):
                                MM(ps[:, q * 128:(q + 1) * 128], w3[:, kc, q * 128:(q + 1) * 128], hc[:, kc, :],
                                   kc == 0, kc == KC - 1, [Rhc, Rwblk[sl]], [Rp], inc=(kc == KC - 1 and q == 3))
                        pi = jb % 2
                        CP(pre[pi][:, :, 0:3], halo[:, jb * 4:(jb + 1) * 4, :], [Rhalo[jb]], [Rpre[pi]], eng="pool")
                        ACT(pre[pi][:, :, 3:131], ps[:].rearrange("p (a b) -> p a b", b=128), AF.Copy, [Rp], [Rpre[pi]])
                        CP(halo[:, jb * 4:(jb + 1) * 4, :], pre[pi][:, :, 128:131], [Rpre[pi]], [Rhalo[jb]], eng="pool")
                        for q in range(4):
                            cc = jb * 4 + q
                            k2 = tc[0] % 2
                            tc[0] += 1
                            ceng = "dve"
                            TS(cacc[k2][:], pre[pi][:, q, 0:128], convw[:, cc * 4:cc * 4 + 1], None, ALU.mult,
                               reads=[Rpre[pi], Rconvw], writes=[Rcacc[k2]], eng=ceng)
                            for k in range(1, 4):
                                STT(cacc[k2][:], pre[pi][:, q, k:k + 128], convw[:, cc * 4 + k:cc * 4 + k + 1], cacc[k2][:],
                                    ALU.mult, ALU.add, [Rpre[pi], Rconvw, Rcacc[k2]], [Rcacc[k2]], eng=ceng)
                            if cc < 12:
                                ACT(csf[k2][:], cacc[k2][:], AF.Silu, [Rcacc[k2], Rconvb], [Rcsf[k2]], bias=convb[:, cc:cc + 1])
                                P3, RP3 = pf[3], Rpf[3]
                                TR(P3[:, q * 128:(q + 1) * 128], csf[k2][:], identf[:], [Rcsf[k2], Ridentf], [RP3])
                                if q == 3:
                                    CP(xs_tok[:, jb * 512:(jb + 1) * 512], P3[:, :], [RP3], [Rxs])
                            elif cc < 16:
                                g = cc - 12
                                ACT(BT[:, g, :], cacc[k2][:], AF.Silu, [Rcacc[k2], Rconvb], [RBT], bias=convb[:, cc:cc + 1])
                                TR(pb[0][:, g * 128:(g + 1) * 128], BT[:, g, :], identb[:], [RBT, Rident], [Rpb[0]])
                                if g == 3:
                                    CP(Btok[:], pb[0][:, 0:512], [Rpb[0]], [RBtok])
                            else:
                                g = cc - 16
                                ACT(CT[:, g, :], cacc[k2][:], AF.Silu, [Rcacc[k2], Rconvb], [RCT], bias=convb[:, cc:cc + 1])
                    else:
                        for hm in range(4):
                            for kc in range(KC):
                                MM(ps[:, hm * 128:(hm + 1) * 128], w3[:, kc, hm * 128:(hm + 1) * 128], hc[:, kc, :],
                                   kc == 0, kc == KC - 1, [Rhc, Rwblk[sl]], [Rp], inc=(kc == KC - 1 and hm == 3))
                        ACT(qmT[:], ps[:].rearrange("p (a b) -> p a b", b=128), AF.Copy, [Rp], [RqmT], scale=SCALE)
                MM(P2[:, 0:24], U_sb[:], la[:], True, True, [RU, Rdt], [RP2], inc=False)
                MM(P2[:, 24:48], onesf[:], la[:], True, True, [Ronesf, Rdt], [RP2], inc=True)
                CP(lacs[:], P2[:, 0:24], [RP2], [Rdt])
                CP(tot[:], P2[:, 24:48], [RP2], [Rdt])
                ACT(expcs[:], lacs[:], AF.Exp, [Rdt], [Rdt])
                TT(dte[:], tot[:], lacs[:], ALU.subtract, [Rdt], [Rdt])
                ACT(dte[:], dte[:], AF.Exp, [Rdt], [Rdt])
                ACT(cdec[:], tot[:], AF.Exp, [Rdt], [Rdt])
                if c + 1 < NT:
                    cn = c + 1
                    norm_tile(x_sb[:, cn, :], Rx[cn], 1, hTc[cn % 2], RhTc[cn % 2], ss[:, cn:cn + 1], rstd[:, cn:cn + 1],
                              Rss[cn], Rrstd[cn])
                for hm in range(4):
                    attention_head(qmT[:, hm, :], RqmT,
                                   lambda kt, hm=hm: kTm[:, hm, kt * 128:(kt + 1) * 128], RkTm,
                                   lambda kt, hm=hm: vm[:, kt, hm * 128:(hm + 1) * 128], Rvm, 128,
                                   lambda a, b_, hm=hm: ycT[:, 12 + hm, a:b_], RycT[12 + hm], pTs, RpTs, rl, Rrl, moba=None)
                v3 = lambda a: a.rearrange("p (a b) -> p a b", b=64)
                b3 = lambda a, n: a.unsqueeze(2).to_broadcast([128, n, 64])
                TT(v3(xdt[:]), v3(xs_tok[:]), b3(dtv[:], 24), ALU.mult, [Rxs, Rdt], [Rxdt])
                TT(dte[:], dte[:], dtv[:], ALU.mult, [Rdt], [Rdt])
                TT(v3(xdte[:]), v3(xs_tok[:]), b3(dte[:], 24), ALU.mult, [Rxs, Rdt], [Rxdte])
                for g in range(4):
                    gi = g % 2
                    MM(P2[:, 128:256], BT[:, g, :], CT[:, g, :], True, True, [RBT, RCT], [RP2])
                    TT(CBm[gi][:], P2[:, 128:256], U_sb[:], ALU.mult, [RP2, RU], [RCBm[gi]])
                    for part, (h0, nh) in enumerate(((0, 4), (4, 2))):
                        bi = part
                        SEG, RSEG = pf[3 + bi], Rpf[3 + bi]
                        for hh in range(nh):
                            h = 6 * g + h0 + hh
                            li = h % 8
                            TS(lh[li][:], SL_sb[:], la[:, h:h + 1], None, ALU.mult, reads=[RSL, Rdt], writes=[Rlh[li]], eng="pool")
                            MM(SEG[:, hh * 128:(hh + 1) * 128], lh[li][:], U_sb[:], True, True, [Rlh[li], RU], [RSEG],
                               inc=(hh == nh - 1))
                        ACT(dec[bi][:, 0:nh * 128], SEG[:, 0:nh * 128], AF.Exp, [RSEG], [Rdec[bi]])
                        TT(scT[bi][:, 0:nh, :], dec[bi][:, 0:nh * 128].rearrange("p (a b) -> p a b", b=128),
                           CBm[gi][:].unsqueeze(1).to_broadcast([128, nh, 128]), ALU.mult, [Rdec[bi], RCBm[gi]], [RscT[bi]])
                        for hh in range(nh):
                            h = 6 * g + h0 + hh
                            hl = h0 + hh
                            MM(pf[5][:, hl * 64:(hl + 1) * 64], scT[bi][:, hh, :], xdt[:, h * 64:(h + 1) * 64], True, True,
                               [RscT[bi], Rxdt], [Rpf[5]], inc=(hl == 5))
                    MM(pf[0][:, 0:384], CT[:, g, :], hbf[:, g, :], True, True, [RCT, Rhbf[g]], [Rpf[0]])
                    MM(pf[1][:, 0:384], Btok[:, g * 128:(g + 1) * 128], xdte[:, g * 384:(g + 1) * 384], True, True,
                       [RBtok, Rxdte], [Rpf[1]])
                    TT(v3(yoff[:]), v3(pf[0][:, 0:384]), b3(expcs[:, 6 * g:6 * g + 6], 6), ALU.mult, [Rpf[0], Rdt], [Ryoff])
                    TT(y_sb[:, g * 384:(g + 1) * 384], pf[5][:, 0:384], yoff[:], ALU.add, [Rpf[5], Ryoff], [Ry])
                    TT(v3(hst[:, g, :]), v3(hst[:, g, :]), b3(cdec[:, 6 * g:6 * g + 6], 6), ALU.mult, [Rhst[g], Rdt], [Rhst[g]])
                    TT(hst[:, g, :], hst[:, g, :], pf[1][:, 0:384], ALU.add, [Rhst[g], Rpf[1]], [Rhst[g]])
                    CP(hbf[:, g, :], hst[:, g, :], [Rhst[g]], [Rhbf[g]], eng="pool")
                TT(v3(ytmp[:]), v3(xs_tok[:]), b3(dskip[:], 24), ALU.mult, [Rxs, Rdskip], [Rytmp])
                TT(y_sb[:], y_sb[:], ytmp[:], ALU.add, [Ry, Rytmp], [Ry])
                TT(y_sb[:], y_sb[:], sz[:], ALU.mult, [Ry, Rsz], [Ry])
                MEMSET(gss[:], 0.0, [Rgss])
                for g in range(4):
                    ACT(junk[:, 0:384], y_sb[:, g * 384:(g + 1) * 384], AF.Square, [Ry, Rgss], [Rgss], accum_out=gss[:, g:g + 1])
                ACT(gss[:], gss[:], AF.Sqrt, [Rgss, Reps], [Rgss], bias=epsc[:, 0:1], scale=1.0 / 384)
                RECIP(gss[:], gss[:], [Rgss], [Rgss])
                g4 = lambda a: a.rearrange("p (a b) -> p a b", b=384)
                TT(g4(ytmp[:]), g4(y_sb[:]), gss[:].unsqueeze(2).to_broadcast([128, 4, 384]), ALU.mult, [Ry, Rgss], [Rytmp])
                TT(un[:], ytmp[:], gssd[:], ALU.mult, [Rytmp, Rgssd], [Run])
                for cc in range(12):
                    bi = 0 if cc < 8 else 1
                    off = cc if cc < 8 else cc - 8
                    TR(pb[bi][:, off * 128:(off + 1) * 128], un[:, cc * 128:(cc + 1) * 128], identb[:], [Run, Rident], [Rpb[bi]],
                       inc=(cc in (7, 11)))
                CP(ycT[:, 0:8, :], pb[0][:].rearrange("p (a b) -> p a b", b=128), [Rpb[0]], RycT[0:8])
                ACT(ycT[:, 8:12, :], pb[1][:, 0:512].rearrange("p (a b) -> p a b", b=128), AF.Copy, [Rpb[1]], RycT[8:12])
                for jb in range(4):
                    u = c * NU + 9 + jb
                    load_unit(u + 2)
                    sl = u % 3
                    w3 = wblk[sl].rearrange("p (a b) -> p a b", b=1024)
                    for q in range(4):
                        cidx = jb * 4 + q
                        for half in range(2):
                            MM(pf[half][:, :], ycT[:, cidx, :], w3[:, q, half * 512:(half + 1) * 512],
                               cidx == 0, cidx == 15, [RycT[cidx], Rwblk[sl]], [Rpf[half]],
                               inc=(q == 3 and half == 1))
                for half in range(2):
                    xs_ = x_sb[:, c, half * 512:(half + 1) * 512]
                    TT(xs_, xs_, pf[half][:, :], ALU.add, [Rpf[half], Rx[c]], [Rx[c]])
            em.barrier()

        def router():
            cv = Carver()
            lg = cv.take([128, 128], F32)
            lg2 = cv.take([128, 128], F32)
            eq = cv.take([128, 128], F32)
            mx1 = cv.take([128, 16], F32)
            mx2 = cv.take([128, 16], F32)
            Rl = R()
            G, RG = pf[0], Rpf[0]
            for t in range(NT):
                for kc in range(KC):
                    MM(G[:, t * 8:(t + 1) * 8], hT[:, kc, t * 128:(t + 1) * 128], wr_sb[:, kc * 8:(kc + 1) * 8],
                       kc == 0, kc == KC - 1, [RhT[t], Rwr], [RG], inc=(kc == KC - 1 and t == NT - 1))
            g3 = lambda a: a.rearrange("p (a b) -> p a b", b=8)
            bc = lambda a: a.unsqueeze(2).to_broadcast([128, 16, 8])
            TT(g3(lg[:]), g3(G[:, 0:128]), br_sb[:].unsqueeze(1).to_broadcast([128, 16, 8]), ALU.add, [RG, Rbr], [Rl])
            RED(mx1[:], g3(lg[:]), ALU.max, [Rl], [Rl])
            TT(g3(eq[:]), g3(lg[:]), bc(mx1[:]), ALU.is_equal, [Rl], [Rl])
            STT(lg2[:], eq[:], -BIG, lg[:], ALU.mult, ALU.add, [Rl], [Rl])
            RED(mx2[:], g3(lg2[:]), ALU.max, [Rl], [Rl])
            TT(g3(eq[:]), g3(lg[:]), bc(mx2[:]), ALU.is_ge, [Rl], [Rl])
            TT(g3(lg2[:]), g3(lg[:]), bc(mx1[:]), ALU.subtract, [Rl], [Rl])
            ACT(lg2[:], lg2[:], AF.Exp, [Rl], [Rl])
            TT(lg2[:], lg2[:], eq[:], ALU.mult, [Rl], [Rl])
            RED(mx1[:], g3(lg2[:]), ALU.add, [Rl], [Rl])
            RECIP(mx1[:], mx1[:], [Rl], [Rl])
            TT(g3(gw[:]), g3(lg2[:]), bc(mx1[:]), ALU.mult, [Rl], [Rgw])
            em.barrier()

        def layer1(s):
            mixer_ssd(s)
            if DBG == "nomoe":
                return
            norm_seq(3)
            router()
            nexp = 9 if not (DBG or "").startswith("moe") else 1 + int(DBG[3:])
            ffn_phase(list(range(1, nexp)), gw=gw[:].rearrange("p (a b) -> p a b", b=8), Rgw=Rgw)

        def final_norm(s):
            cv = Carver()
            ot = [cv.take([128, D], F32) for _ in range(2)]
            Rot = [R(), R()]
            for t in range(NT):
                i = t % 2
                MEMSET(ss[:, t:t + 1], 0.0, [Rss[t]])
                ACT(junk[:], x_sb[:, t, :], AF.Square, [Rx[t], Rss[t]], [Rss[t]], accum_out=ss[:, t:t + 1])
                ACT(rstd[:, t:t + 1], ss[:, t:t + 1], AF.Sqrt, [Rss[t], Reps], [Rrstd[t]], bias=epsc[:, 0:1], scale=1.0 / D)
                RECIP(rstd[:, t:t + 1], rstd[:, t:t + 1], [Rrstd[t]], [Rrstd[t]])
                STT(ot[i][:], x_sb[:, t, :], rstd[:, t:t + 1], gfin[:], ALU.mult, ALU.mult,
                    [Rx[t], Rrstd[t], Rgfin], [Rot[i]])
                em.dma("sp", "out", out_d[s, t * 128:(t + 1) * 128, :], ot[i][:], reads=[Rot[i]], writes=[Rout])
            em.barrier()

        Rout = R()

        wl1_bf = nc.dram_tensor("wl1_bf", [13, 128, 4096], BF16, kind="Internal").ap()
        Rwl1 = [R() for _ in range(13)]
        for j in range(13):
            src = wssd_d[j] if j < 9 else wout_d[1][:, (j - 9) * 4096:(j - 8) * 4096]
            em.dma("pool", "wl1cast%d" % (j % 4), wl1_bf[j], src, writes=[Rwl1[j]])

        for s in range(n_seq):
            for t4 in range(4):
                em.dma("sp", "xload%d" % t4, x_sb[:, t4 * 4:(t4 + 1) * 4, :],
                       x_d[s, t4 * 512:(t4 + 1) * 512, :].rearrange("(t p) d -> p t d", p=128),
                       writes=Rx[t4 * 4:(t4 + 1) * 4])
            norm_seq(0)
            mixer_moba(s)
            norm_seq(2)
            ffn_phase([0])
            if n_layers > 1:
                layer1(s)
            final_norm(s)

        em.finish()
    return nc


_SHARED_CACHE = {}


def _run(inputs, n_layers=2, n_seq=2, n_cores=8, core0=0):
    inp = {k: np.asarray(v) for k, v in inputs.items()}
    sh = _prep_shared(inp)
    nc = build(n_layers=n_layers, n_seq=n_seq)
    in_maps = []
    for c in range(core0, core0 + n_cores):
        m = dict(sh)
        m["x"] = np.ascontiguousarray(inp["x"][n_seq * c:n_seq * (c + 1)], dtype=np.float32)
        m["mem"] = np.ascontiguousarray(inp["mem"][n_seq * c:n_seq * (c + 1)], dtype=np.float32)
        in_maps.append(m)
    res = run_bass_kernel_spmd(nc, in_maps, core_ids=list(range(n_cores)))
    return np.concatenate([np.asarray(r["out"]) for r in res.results], axis=0)


N_CORES = 8
SEQ_PER_CORE = 16 // N_CORES


def kernel(**inputs):
    return _run(inputs, n_layers=2, n_seq=SEQ_PER_CORE, n_cores=N_CORES).astype(np.float32)
```

```python
import math
from contextlib import ExitStack
import numpy as np
import concourse.bass as bass
import concourse.mybir as mybir
from concourse.bass_utils import run_bass_kernel_spmd

F32 = mybir.dt.float32
BF16 = mybir.dt.bfloat16
AF = mybir.ActivationFunctionType
ALU = mybir.AluOpType
AX = mybir.AxisListType

S = 2048
D = 1024
NT = 16
KC = 8
EPS = 1e-6
SCALE = 128 ** -0.5
NEG = -30000.0
BIG = 1.0e30
DFF = 3584
NFB = 7
NEXP = 8
DBG = None
PACE_N = 512


class R:
    __slots__ = ("w", "r")

    def __init__(self):
        self.w = None
        self.r = []


class Emitter:
    ENG = ("pe", "act", "dve", "pool", "sp")

    def __init__(self, nc, stack):
        self.nc = nc
        self.stack = stack
        self.q = {e: [] for e in self.ENG}
        self.sems = {}
        self.cnt = {}
        self.waited = {}
        for e in self.ENG:
            self._mksem(e)
        self.n_instr = 0

    def _mksem(self, key):
        self.sems[key] = self.stack.enter_context(self.nc.semaphore("s_" + key))
        self.cnt[key] = 0

    def sbuf(self, name, shape, dt):
        return self.stack.enter_context(self.nc.sbuf_tensor("sb_" + name, list(shape), dt))

    def psum(self, name, shape, dt=F32):
        return self.stack.enter_context(self.nc.psum_tensor("ps_" + name, list(shape), dt))

    def _deps(self, eng, reads, writes):
        deps = {}

        def add(t):
            if t is None:
                return
            k, v = t
            if k == "pe" and eng == "pe":
                return
            if deps.get(k, 0) < v:
                deps[k] = v
        for r in reads:
            add(r.w)
        for w in writes:
            add(w.w)
            for t in w.r:
                add(t)
        out = []
        for k, v in deps.items():
            if self.waited.get((eng, k), 0) >= v:
                continue
            self.waited[(eng, k)] = v
            out.append((self.sems[k], v))
        return out

    def _mark(self, ticket, reads, writes):
        for r in reads:
            r.r.append(ticket)
            if len(r.r) > 48:
                best = {}
                for k, v in r.r:
                    if best.get(k, 0) < v:
                        best[k] = v
                r.r = list(best.items())
        for w in writes:
            w.w = ticket
            w.r = []

    def op(self, eng, fn, reads=(), writes=(), inc=True):
        waits = self._deps(eng, reads, writes)
        ticket = (eng, self.cnt[eng] + 1)
        if inc:
            self.cnt[eng] += 1
        sem = self.sems[eng]
        self._mark(ticket, reads, writes)
        self.n_instr += 1

        def thunk(e):
            for s, v in waits:
                e.wait_ge(s, v)
            ins = fn(e)
            if inc:
                ins.then_inc(sem, 1)
        self.q[eng].append(thunk)
        return ticket

    def dma(self, queue, slot, out, in_, reads=(), writes=()):
        key = "d_" + slot + "_" + queue
        if key not in self.sems:
            self._mksem(key)
        waits = self._deps(queue, reads, writes)
        self.cnt[key] += 16
        ticket = (key, self.cnt[key])
        sem = self.sems[key]
        self._mark(ticket, reads, writes)
        self.n_instr += 1

        def thunk(e):
            for s, v in waits:
                e.wait_ge(s, v)
            e.dma_start(out=out, in_=in_).then_inc(sem, 16)
        self.q[queue].append(thunk)
        return ticket

    def barrier(self):
        for eng in self.ENG:
            waits = []
            for k, v in self.cnt.items():
                if v == 0 or k == eng and eng in ("pe",):
                    continue
                if self.waited.get((eng, k), 0) >= v:
                    continue
                self.waited[(eng, k)] = v
                waits.append((self.sems[k], v))
            if waits:
                def thunk(e, waits=waits):
                    for s, v in waits:
                        e.wait_ge(s, v)
                self.q[eng].append(thunk)

    def finish(self):
        nc = self.nc
        q = self.q
        with nc.Block() as block:
            @block.tensor
            def _(e):
                for f in q["pe"]:
                    f(e)

            @block.scalar
            def _(e):
                for f in q["act"]:
                    f(e)

            @block.vector
            def _(e):
                for f in q["dve"]:
                    f(e)

            @block.gpsimd
            def _(e):
                for f in q["pool"]:
                    f(e)

            @block.sync
            def _(e):
                for f in q["sp"]:
                    f(e)


def _t5_bucket_np(dist):
    n = np.maximum(dist, 0).astype(np.int32)
    max_exact = 16
    nf = np.maximum(n, 1).astype(np.float32)
    large = max_exact + (np.log(nf / np.float32(max_exact)) / np.float32(math.log(128 / max_exact))
                         * np.float32(32 - max_exact)).astype(np.int32)
    large = np.minimum(large, 31)
    return np.where(n < max_exact, n, large)


def _consts():
    c = {}
    i = np.arange(128)
    c["identf"] = np.eye(128, dtype=np.float32)
    c["U"] = (i[:, None] <= i[None, :]).astype(np.float32)
    c["SL"] = (i[:, None] > i[None, :]).astype(np.float32)
    c["causneg"] = np.where(i[:, None] > i[None, :], NEG, 0.0).astype(np.float32)
    E = np.zeros((8, 8 * 128), np.float32)
    for b in range(8):
        E[b, b * 128:(b + 1) * 128] = 1.0
    c["E"] = E
    tb = np.arange(16)[:, None] // 2
    bb = np.arange(8)[None, :]
    past = (bb < tb).astype(np.float32).reshape(1, 128)
    own = (bb == tb).astype(np.float32).reshape(1, 128)
    c["past01"] = np.repeat(past, 128, 0)
    c["own01"] = np.repeat(own, 128, 0)
    c["pastneg"] = np.repeat(np.where(past > 0, 0.0, -BIG).astype(np.float32), 128, 0)
    return c


def _feat_major(v):
    n = v.shape[0] // 128
    return np.ascontiguousarray(v.reshape(n, 128).T)


def _bcast(v):
    return np.ascontiguousarray(np.broadcast_to(v.reshape(1, -1), (128, v.size)))


def _kmajor(w):
    k, n = w.shape
    return np.ascontiguousarray(w.reshape(k // 128, 128, n).transpose(1, 0, 2))


def _prep_shared(inp):
    f = np.float32
    sh = {}
    w0 = inp["w_in_moba"][0]
    wq = _kmajor(w0[:, 0:1536]).reshape(128, 8, 12, 128)
    wk = _kmajor(w0[:, 1536:3072]).reshape(128, 8, 12, 128)
    wv = _kmajor(w0[:, 3072:4608]).reshape(128, 8, 12, 128)
    wqkv = np.stack([wq, wk, wv], axis=3)
    sh["wqkv0"] = np.ascontiguousarray(wqkv.transpose(2, 0, 1, 3, 4)).reshape(12, 128, 8 * 384)
    wqm = _kmajor(w0[:, 4608:5120]).reshape(128, 8, 4, 128)
    sh["wqm0"] = np.ascontiguousarray(wqm.transpose(2, 0, 1, 3)).reshape(4, 128, 8 * 128)
    sh["wkv"] = np.stack([_kmajor(inp["w_mem_kv"][i]) for i in range(2)]).reshape(2, 128, 8 * 1024)
    sh["wout"] = np.stack([_kmajor(inp["w_out"][i]) for i in range(2)]).reshape(2, 128, 16 * 1024)
    wg = np.concatenate([inp["w_ffn_gate"], inp["w_exp_gate"][0]], 0)
    wu = np.concatenate([inp["w_ffn_up"], inp["w_exp_up"][0]], 0)
    wd = np.concatenate([inp["w_ffn_down"], inp["w_exp_down"][0]], 0)
    wgk = wg.reshape(9, 8, 128, NFB, 512).transpose(0, 3, 2, 1, 4)
    wuk = wu.reshape(9, 8, 128, NFB, 512).transpose(0, 3, 2, 1, 4)
    sh["wgu"] = np.ascontiguousarray(np.stack([wgk, wuk], axis=4)).reshape(9, NFB, 128, 8 * 2 * 512)
    sh["wd"] = np.ascontiguousarray(wd.reshape(9, NFB, 4, 128, 1024).transpose(0, 1, 3, 2, 4)).reshape(9, NFB, 128, 4 * 1024)
    w1 = inp["w_in_ssd"][0]
    blocks = [w1[:, j * 512:(j + 1) * 512] for j in range(8)] + [w1[:, 4120:4632]]
    sh["wssd"] = np.stack([_kmajor(b) for b in blocks]).reshape(9, 128, 8 * 512)
    sh["wdt"] = _kmajor(w1[:, 4096:4120]).reshape(128, 8 * 24)
    gv = np.stack([_feat_major(inp["g_mix"][0]), _feat_major(inp["g_mix"][1]),
                   _feat_major(inp["g_ffn"][0]), _feat_major(inp["g_ffn"][1]),
                   _feat_major(inp["g_mem"][0]), _feat_major(inp["g_mem"][1])], axis=1)
    sh["gvec"] = np.ascontiguousarray(gv).reshape(128, 48)
    sh["gfin"] = _bcast(inp["g_final"])
    cw = inp["conv_w"][0]
    sh["convw"] = np.ascontiguousarray(cw.reshape(4, 20, 128).transpose(2, 1, 0)).reshape(128, 80)
    sh["convb"] = _feat_major(inp["conv_b"][0])
    sh["dtb"] = _bcast(inp["dt_bias"][0])
    sh["alog"] = _bcast(inp["a_log"][0])
    sh["dskip"] = _bcast(inp["d_skip"][0])
    sh["gssd"] = _bcast(inp["g_ssd_out"][0])
    sh["wr"] = _kmajor(inp["w_router"][0]).reshape(128, 64)
    sh["br"] = _bcast(inp["b_router"][0])
    rb = inp["rel_bias"]
    i = np.arange(128)
    d0 = i[None, :] - i[:, None]
    bk0 = _t5_bucket_np(d0)
    bk1 = _t5_bucket_np(d0 + 128)
    tt = np.stack([rb[bk0], rb[bk1]], 0)
    sh["ttg"] = np.ascontiguousarray(tt.transpose(1, 0, 3, 2)).reshape(128, 2 * 12 * 128)
    sh["b31"] = _bcast(rb[31])
    sh.update(_consts())
    return {k: np.ascontiguousarray(v, dtype=f) for k, v in sh.items()}


def build(n_layers=2, n_seq=2):
    nc = bass.Bass("TRN2", target_bir_lowering=False)

    def din(name, shape):
        return nc.dram_tensor(name, list(shape), F32, kind="ExternalInput").ap()

    x_d = din("x", [n_seq, S, D])
    mem_d = din("mem", [n_seq, 256, D])
    wqkv0_d = din("wqkv0", [12, 128, 8 * 384])
    wqm0_d = din("wqm0", [4, 128, 8 * 128])
    wkv_d = din("wkv", [2, 128, 8 * 1024])
    wout_d = din("wout", [2, 128, 16 * 1024])
    wgu_d = din("wgu", [9, NFB, 128, 8 * 2 * 512])
    wd_d = din("wd", [9, NFB, 128, 4 * 1024])
    wssd_d = din("wssd", [9, 128, 8 * 512])
    wdt_d = din("wdt", [128, 8 * 24])
    gvec_d = din("gvec", [128, 48])
    gfin_d = din("gfin", [128, D])
    convw_d = din("convw", [128, 80])
    convb_d = din("convb", [128, 20])
    dtb_d = din("dtb", [128, 24])
    alog_d = din("alog", [128, 24])
    dskip_d = din("dskip", [128, 24])
    gssd_d = din("gssd", [128, 1536])
    wr_d = din("wr", [128, 64])
    br_d = din("br", [128, 8])
    ttg_d = din("ttg", [128, 2 * 12 * 128])
    b31_d = din("b31", [128, 12])
    identf_d = din("identf", [128, 128])
    U_d = din("U", [128, 128])
    SL_d = din("SL", [128, 128])
    causneg_d = din("causneg", [128, 128])
    E_d = din("E", [8, 1024])
    past01_d = din("past01", [128, 128])
    own01_d = din("own01", [128, 128])
    pastneg_d = din("pastneg", [128, 128])
    out_d = nc.dram_tensor("out", [n_seq, S, D], F32, kind="ExternalOutput").ap()

    with ExitStack() as st:
        em = Emitter(nc, st)

        def MM(out, lhsT, rhs, start, stop, reads, writes, inc=None):
            if inc is None:
                inc = stop
            em.op("pe", lambda e: e.matmul(out, lhsT=lhsT, rhs=rhs, start=start, stop=stop),
                  reads, writes, inc)

        def TR(out, in_, ident, reads, writes, inc=True):
            em.op("pe", lambda e: e.transpose(out=out, in_=in_, identity=ident), reads, writes, inc)

        def ACT(out, in_, func, reads, writes, bias=None, scale=None, accum_out=None):
            kw = {}
            if bias is not None:
                kw["bias"] = bias
            if scale is not None:
                kw["scale"] = scale
            if accum_out is not None:
                kw["accum_out"] = accum_out
            em.op("act", lambda e: e.activation(out=out, in_=in_, func=func, **kw), reads, writes)

        def TT(out, in0, in1, op, reads, writes, eng="dve"):
            em.op(eng, lambda e: e.tensor_tensor(out=out, in0=in0, in1=in1, op=op), reads, writes)

        def TS(out, in0, s1, s2, op0, op1=None, reads=(), writes=(), eng="dve", accum_out=None):
            kw = {}
            if op1 is not None:
                kw["op1"] = op1
            if accum_out is not None:
                kw["accum_out"] = accum_out
            em.op(eng, lambda e: e.tensor_scalar(out=out, in0=in0, scalar1=s1, scalar2=s2, op0=op0, **kw),
                  reads, writes)

        def STT(out, in0, scalar, in1, op0, op1, reads, writes, eng="dve"):
            em.op(eng, lambda e: e.scalar_tensor_tensor(out=out, in0=in0, scalar=scalar, in1=in1,
                                                        op0=op0, op1=op1), reads, writes)

        def CP(out, in_, reads, writes, eng="dve"):
            em.op(eng, lambda e: e.tensor_copy(out=out, in_=in_), reads, writes)

        def RED(out, in_, op, reads, writes, eng="dve"):
            em.op(eng, lambda e: e.tensor_reduce(out=out, in_=in_, axis=AX.X, op=op), reads, writes)

        def RECIP(out, in_, reads, writes):
            em.op("dve", lambda e: e.reciprocal(out=out, in_=in_), reads, writes)

        def MEMSET(ap, val, writes, eng="pool", reads=()):
            em.op(eng, lambda e: e.memset(ap, val), reads, writes)

        x_sb = em.sbuf("x_sb", [128, NT, D], F32)
        Rx = [R() for _ in range(NT)]
        RhT = [R() for _ in range(NT)]

        def cload(name, src, shape, dt=F32, queue=None):
            t = em.sbuf(name, shape, dt)
            r = R()
            q = queue or ("pool" if dt != F32 else "sp")
            flat = t[:] if len(shape) == 2 else t[:]
            em.dma(q, "const", flat, src, writes=[r])
            return t, r

        identb, Rident = cload("identb", identf_d, [128, 128], BF16)
        identf, Ridentf = cload("identf", identf_d, [128, 128])
        U_sb, RU = cload("U_sb", U_d, [128, 128])
        SL_sb, RSL = cload("SL_sb", SL_d, [128, 128])
        causneg, Rcaus = cload("causneg", causneg_d, [128, 128])
        E_sb, RE = cload("E_sb", E_d, [8, 1024], BF16)
        past01, Rpast = cload("past01", past01_d, [128, 128])
        own01, Rown = cload("own01", own01_d, [128, 128])
        pastneg, Rpastneg = cload("pastneg", pastneg_d, [128, 128])
        gvec, Rgvec = cload("gvec", gvec_d, [128, 48])
        gfin, Rgfin = cload("gfin", gfin_d, [128, D])
        b31, Rb31 = cload("b31", b31_d, [128, 12])
        onesb = em.sbuf("onesb", [128, 128], BF16)
        Rones = R()
        MEMSET(onesb[:], 1.0, [Rones])
        onesf = em.sbuf("onesf", [128, 128], F32)
        Ronesf = R()
        MEMSET(onesf[:], 1.0, [Ronesf])
        junk = em.sbuf("junk", [128, D], BF16)
        epsc = em.sbuf("epsc", [128, 1], F32)
        Reps = R()
        MEMSET(epsc[:], EPS, [Reps])
        ss = em.sbuf("ss", [128, NT], F32)
        rstd = em.sbuf("rstd", [128, NT], F32)
        Rss = [R() for _ in range(NT)]
        Rrstd = [R() for _ in range(NT)]
        xn = [em.sbuf("xn%d" % i, [128, D], BF16) for i in range(2)]
        Rxn = [R(), R()]

        wdt_sb, Rwdt = cload("wdt_sb", wdt_d, [128, 192], BF16)
        convw, Rconvw = cload("convw_sb", convw_d, [128, 80])
        convb, Rconvb = cload("convb_sb", convb_d, [128, 20])
        dtb, Rdtb = cload("dtb_sb", dtb_d, [128, 24])
        a_bc, Ra_bc = cload("a_bc", alog_d, [128, 24])
        dskip, Rdskip = cload("dskip_sb", dskip_d, [128, 24])
        wr_sb, Rwr = cload("wr_sb", wr_d, [128, 64], BF16)
        br_sb, Rbr = cload("br_sb", br_d, [128, 8])
        gw = em.sbuf("gw", [128, 128], F32)
        Rgw = R()
        onec = em.sbuf("onec", [128, 1], F32)
        Ronec = R()
        MEMSET(onec[:], 1.0, [Ronec])

        TTb = em.sbuf("TTb", [128, 2 * 12 * 128], BF16)
        RTTb = R()

        pf = [em.psum("pf%d" % i, [128, 512], F32) for i in range(6)]
        Rpf = [R() for _ in range(6)]
        pb = [em.psum("pb%d" % i, [128, 1024], BF16) for i in range(2)]
        Rpb = [R(), R()]

        ARENA = 86 * 1024
        ARENA_FULL = ARENA + 32 * 1024
        arena = em.sbuf("arena", [128, ARENA_FULL // 2], BF16)
        hT = arena[:, ARENA // 2: ARENA_FULL // 2].rearrange("p (a b) -> p a b", b=S)

        class Carver:
            def __init__(self, limit=None):
                self.off = 0
                self.limit = limit or ARENA

            def take(self, shape, dt, parts=128):
                esz = 4 if dt == F32 else 2
                n = int(np.prod(shape[1:]))
                nbytes = (n * esz + 31) // 32 * 32
                assert self.off + nbytes <= self.limit, ("arena overflow", self.off, nbytes)
                ap = arena[0:shape[0], self.off // 2: self.off // 2 + n * esz // 2]
                if dt == F32:
                    ap = ap.bitcast(F32)
                self.off += nbytes
                if len(shape) == 3:
                    ap = ap.rearrange("p (a b) -> p a b", b=shape[2])
                elif len(shape) == 4:
                    ap = ap.rearrange("p (a b c) -> p a b c", b=shape[2], c=shape[3])
                return ap

        def setup_bias():
            cv = Carver()
            ttg = cv.take([128, 2 * 12 * 128], F32)
            Rttg = R()
            em.dma("sp", "const", ttg, ttg_d, writes=[Rttg])
            em.barrier()
            ACT(a_bc[:], a_bc[:], AF.Exp, [Ra_bc], [Ra_bc])
            TS(a_bc[:], a_bc[:], -1.0, None, ALU.mult, reads=[Ra_bc], writes=[Ra_bc])
            for j in range(2):
                for h in range(12):
                    sl = slice((j * 12 + h) * 128, (j * 12 + h + 1) * 128)
                    if j == 0:
                        STT(TTb[:, sl], ttg[:, sl], b31[:, h:h + 1], causneg[:], ALU.subtract, ALU.add,
                            [Rttg, Rb31, Rcaus], [RTTb])
                    else:
                        TS(TTb[:, sl], ttg[:, sl], b31[:, h:h + 1], None, ALU.subtract,
                           reads=[Rttg, Rb31], writes=[RTTb])
            em.barrier()

        setup_bias()

        nrm_ctr = [0]

        def norm_tile(src_ap, Rsrc, gi, dst_ap, Rdst, ss_ap, rstd_ap, Rs, Rr, ncols=D):
            i = nrm_ctr[0] % 2
            nrm_ctr[0] += 1
            MEMSET(ss_ap, 0.0, [Rs])
            ACT(junk[:], src_ap, AF.Square, [Rsrc, Rs], [Rs], accum_out=ss_ap)
            ACT(rstd_ap, ss_ap, AF.Sqrt, [Rs, Reps], [Rr], bias=epsc[:, 0:1], scale=1.0 / D)
            RECIP(rstd_ap, rstd_ap, [Rr], [Rr])
            TS(xn[i][:], src_ap, rstd_ap, None, ALU.mult, reads=[Rsrc, Rr], writes=[Rxn[i]])
            for kc in range(KC):
                TR(pb[i][:, kc * 128:(kc + 1) * 128], xn[i][:, kc * 128:(kc + 1) * 128], identb[:],
                   [Rxn[i], Rident], [Rpb[i]], inc=(kc == KC - 1))
            gb = gvec[:, gi * 8:(gi + 1) * 8].unsqueeze(2).to_broadcast([128, 8, 128])
            TT(dst_ap, pb[i][:].rearrange("p (a b) -> p a b", b=128), gb, ALU.mult,
               [Rpb[i], Rgvec], [Rdst])

        def norm_seq(gi):
            for t in range(NT):
                norm_tile(x_sb[:, t, :], Rx[t], gi, hT[:, :, t * 128:(t + 1) * 128], RhT[t],
                          ss[:, t:t + 1], rstd[:, t:t + 1], Rss[t], Rrstd[t])

        pT_ctr = [0]
        s_ctr = [0]

        def attention_head(qT_ap, RqT, kT_fn, RkT, V_fn, RV, n_q, out_fn, Rout, pTs, RpTs, rl, Rrl,
                           moba=None):
            O, L = pf[4], pf[5]
            RO, RL = Rpf[4], Rpf[5]
            ngrp = (n_q + 511) // 512
            pending = None

            def emit_pv(kt, c0, qn, pi, first, last):
                MM(O[:, c0:qn], V_fn(kt), pTs[pi][:, c0:qn], first, last, [RV, RpTs[pi]], [RO], inc=False)
                MM(L[:, c0:qn], onesb[:], pTs[pi][:, c0:qn], first, last, [Rones, RpTs[pi]], [RL], inc=True)

            for qg in range(ngrp):
                q0 = qg * 512
                qn = min(512, n_q - q0)
                if moba is None:
                    kts = [0, 1]
                else:
                    kts = list(range(0, 4 * qg + 4))
                for idx, kt in enumerate(kts):
                    if moba is None:
                        c0 = 0
                    else:
                        c0 = max(0, kt - 4 * qg) * 128
                    si = s_ctr[0] % 2
                    s_ctr[0] += 1
                    Sb, RS = pf[2 + si], Rpf[2 + si]
                    extra = []
                    if moba is not None:
                        b = kt // 2
                        extra.append((E_sb[0:8, b * 128:(b + 1) * 128], moba["MnegT"][0:8, q0 + c0:q0 + qn],
                                      c0, qn, [RE, moba["RM"]]))
                        for j in range(2):
                            qt = kt + j
                            cc = (qt - 4 * qg) * 128
                            if 0 <= cc < qn and cc >= c0:
                                sl = slice((j * 12 + moba["h"]) * 128, (j * 12 + moba["h"] + 1) * 128)
                                extra.append((identb[:], TTb[:, sl], cc, cc + 128, [Rident, RTTb]))
                    MM(Sb[:, c0:qn], kT_fn(kt), qT_ap[:, q0 + c0:q0 + qn], True, len(extra) == 0,
                       [RkT, RqT], [RS])
                    for ei, (l_, r_, a0, a1, rr) in enumerate(extra):
                        MM(Sb[:, a0:a1], l_, r_, False, ei == len(extra) - 1, rr, [RS])
                    pi = pT_ctr[0] % len(pTs)
                    pT_ctr[0] += 1
                    ACT(pTs[pi][:, c0:qn], Sb[:, c0:qn], AF.Exp, [RS], [RpTs[pi]])
                    if pending is not None:
                        emit_pv(*pending)
                    pending = (kt, c0, qn, pi, idx == 0, idx == len(kts) - 1)
                emit_pv(*pending)
                pending = None
                RECIP(rl[:, 0:qn], L[:, 0:qn], [RL], [Rrl])
                TT(out_fn(q0, q0 + qn), O[:, 0:qn], rl[:, 0:qn], ALU.mult, [RO, Rrl], [Rout])

        def mem_kv(s, layer, cv, kTm, RkTm, vm, Rvm):
            memx = cv.take([128, 2, D], F32)
            Rmemx = [R(), R()]
            memnT = cv.take([128, KC, 256], BF16)
            RmemnT = [R(), R()]
            wkv = cv.take([128, KC, 1024], BF16)
            Rwkv = R()
            mss = cv.take([128, 2], F32)
            mrs = cv.take([128, 2], F32)
            Rm1 = [R(), R()]
            Rm2 = [R(), R()]
            em.dma("pool", "wkv", wkv.rearrange("p a b -> p (a b)"), wkv_d[layer], writes=[Rwkv])
            for mt in range(2):
                em.dma("sp", "memx%d" % mt, memx[:, mt, :], mem_d[s, mt * 128:(mt + 1) * 128, :], writes=[Rmemx[mt]])
            for mt in range(2):
                norm_tile(memx[:, mt, :], Rmemx[mt], 4 + layer, memnT[:, :, mt * 128:(mt + 1) * 128], RmemnT[mt],
                          mss[:, mt:mt + 1], mrs[:, mt:mt + 1], Rm1[mt], Rm2[mt])
            for hm in range(4):
                ps, Rp = pf[hm % 2], Rpf[hm % 2]
                for kc in range(KC):
                    MM(ps[:, 0:256], wkv[:, kc, hm * 128:(hm + 1) * 128], memnT[:, kc, :], kc == 0, kc == KC - 1,
                       [Rwkv] + RmemnT, [Rp])
                CP(kTm[:, hm, :], ps[:, 0:256], [Rp], [RkTm])
            for mt in range(2):
                ps, Rp = pf[mt % 2], Rpf[mt % 2]
                for kc in range(KC):
                    MM(ps[:, :], memnT[:, kc, mt * 128:(mt + 1) * 128], wkv[:, kc, 512:1024], kc == 0, kc == KC - 1,
                       [Rwkv, RmemnT[mt]], [Rp])
                ACT(vm[:, mt, :], ps[:, :], AF.Copy, [Rp], [Rvm])

        def mixer_moba(s):
            cv = Carver()
            kTm = cv.take([128, 4, 256], BF16)
            RkTm = R()
            vm = cv.take([128, 2, 512], BF16)
            Rvm = R()
            mark = cv.off
            mem_kv(s, 0, cv, kTm, RkTm, vm, Rvm)
            em.barrier()
            cv.off = mark
            wq = [cv.take([128, KC, 384], BF16) for _ in range(2)]
            Rwq = [R(), R()]
            qT = [cv.take([128, S], BF16) for _ in range(2)]
            RqT = [R(), R()]
            kT = [cv.take([128, S], BF16) for _ in range(2)]
            RkT = [R(), R()]
            Vh = [cv.take([128, NT, 128], BF16) for _ in range(2)]
            RV = [R(), R()]
            oTg = cv.take([128, 4, S], BF16)
            RoTg = [R() for _ in range(4)]
            woutg = cv.take([128, 4, D], BF16)
            Rwoutg = R()
            pTs = [cv.take([128, 512], BF16) for _ in range(3)]
            RpTs = [R() for _ in range(3)]
            rl = cv.take([128, 512], F32)
            Rrl = R()
            gm = cv.take([128, 128], F32)
            gm2 = cv.take([128, 128], F32)
            eq = cv.take([128, 128], F32)
            mx = cv.take([128, 16], F32)
            Rg = R()
            Mneg = cv.take([128, 128], BF16)
            RMneg = R()
            MnegT = cv.take([8, S], BF16, parts=8)
            RMnegT = R()
            km = cv.take([128, 8], F32)
            kmb = cv.take([128, 8], BF16)
            Rkm = R()

            def load_w(h):
                sl = h % 2
                if h < 12:
                    em.dma("pool", "wq%d" % sl, wq[sl].rearrange("p a b -> p (a b)"), wqkv0_d[h], writes=[Rwq[sl]])
                else:
                    em.dma("pool", "wq%d" % sl, wq[sl][:, :, 0:128], wqm0_d[h - 12].rearrange("p (a b) -> p a b", b=128),
                           writes=[Rwq[sl]])

            load_w(0)
            pj = [0]

            def proj_fm(dst, Rdst, w, Rw, c0, scale, use_act):
                for tg in range(4):
                    i = pj[0] % 2
                    pj[0] += 1
                    ps, Rp = pf[i], Rpf[i]
                    for kc in range(KC):
                        MM(ps[:, :], w[:, kc, c0:c0 + 128], hT[:, kc, tg * 512:(tg + 1) * 512], kc == 0, kc == KC - 1,
                           [Rw] + RhT[tg * 4:tg * 4 + 4], [Rp])
                    if use_act:
                        ACT(dst[:, tg * 512:(tg + 1) * 512], ps[:, :], AF.Copy, [Rp], [Rdst], scale=scale)
                    else:
                        CP(dst[:, tg * 512:(tg + 1) * 512], ps[:, :], [Rp], [Rdst])

            for h in range(16):
                sl = h % 2
                grp, hh = h // 4, h % 4
                if h + 1 < 16:
                    load_w(h + 1)
                if hh == 0:
                    em.dma("pool", "woutg", woutg.rearrange("p a b -> p (a b)"),
                           wout_d[0][:, grp * 4096:(grp + 1) * 4096], writes=[Rwoutg])
                proj_fm(qT[sl], RqT[sl], wq[sl], Rwq[sl], 0, SCALE, True)
                if h < 12:
                    proj_fm(kT[sl], RkT[sl], wq[sl], Rwq[sl], 128, None, False)
                    for t4 in range(4):
                        i = pj[0] % 2
                        pj[0] += 1
                        ps, Rp = pf[i], Rpf[i]
                        for j in range(4):
                            t = t4 * 4 + j
                            for kc in range(KC):
                                MM(ps[:, j * 128:(j + 1) * 128], hT[:, kc, t * 128:(t + 1) * 128], wq[sl][:, kc, 256:384],
                                   kc == 0, kc == KC - 1, [Rwq[sl], RhT[t]], [Rp], inc=(kc == KC - 1 and j == 3))
                        ACT(Vh[sl][:, t4 * 4:(t4 + 1) * 4, :], ps[:].rearrange("p (a b) -> p a b", b=128), AF.Copy,
                            [Rp], [RV[sl]])
                    RED(km[:], kT[sl].rearrange("p (a b) -> p a b", b=256), ALU.add, [RkT[sl]], [Rkm])
                    TS(kmb[:], km[:], 1.0 / 256, None, ALU.mult, reads=[Rkm], writes=[Rkm])
                    G, RG = pf[0], Rpf[0]
                    pj[0] = 1
                    for t in range(NT):
                        MM(G[:, t * 8:(t + 1) * 8], qT[sl][:, t * 128:(t + 1) * 128], kmb[:], True, True,
                           [RqT[sl], Rkm], [RG], inc=(t == NT - 1))
                    g3 = lambda a: a.rearrange("p (a b) -> p a b", b=8)
                    mb = lambda: mx[:].unsqueeze(2).to_broadcast([128, 16, 8])
                    TT(gm[:], G[:, 0:128], pastneg[:], ALU.add, [RG, Rpastneg], [Rg])
                    RED(mx[:], g3(gm[:]), ALU.max, [Rg], [Rg])
                    TT(g3(eq[:]), g3(gm[:]), mb(), ALU.is_equal, [Rg], [Rg])
                    STT(gm2[:], eq[:], -BIG, gm[:], ALU.mult, ALU.add, [Rg], [Rg])
                    RED(mx[:], g3(gm2[:]), ALU.max, [Rg], [Rg])
                    TT(g3(eq[:]), g3(gm2[:]), mb(), ALU.is_equal, [Rg], [Rg])
                    STT(gm2[:], eq[:], -BIG, gm2[:], ALU.mult, ALU.add, [Rg], [Rg])
                    RED(mx[:], g3(gm2[:]), ALU.max, [Rg], [Rg])
                    TT(g3(eq[:]), g3(gm[:]), mb(), ALU.is_ge, [Rg], [Rg])
                    TT(eq[:], eq[:], past01[:], ALU.mult, [Rg, Rpast], [Rg])
                    TT(eq[:], eq[:], own01[:], ALU.add, [Rg, Rown], [Rg])
                    TS(Mneg[:], eq[:], -1.0, -NEG, ALU.add, ALU.mult, reads=[Rg], writes=[RMneg])
                    for half in range(2):
                        pbi = half
                        for t8 in range(8):
                            t = half * 8 + t8
                            TR(pb[pbi][0:8, t8 * 128:(t8 + 1) * 128], Mneg[:, t * 8:(t + 1) * 8], identb[:],
                               [RMneg, Rident], [Rpb[pbi]], inc=(t8 == 7))
                        ACT(MnegT[0:8, half * 1024:(half + 1) * 1024], pb[pbi][0:8, :], AF.Copy, [Rpb[pbi]], [RMnegT])
                    attention_head(qT[sl], RqT[sl],
                                   lambda kt, sl=sl: kT[sl][:, kt * 128:(kt + 1) * 128], RkT[sl],
                                   lambda kt, sl=sl: Vh[sl][:, kt, :], RV[sl], S,
                                   lambda a, b_, hh=hh: oTg[:, hh, a:b_], RoTg[hh], pTs, RpTs, rl, Rrl,
                                   moba=dict(h=h, MnegT=MnegT, RM=RMnegT))
                else:
                    hm = h - 12
                    attention_head(qT[sl], RqT[sl],
                                   lambda kt, hm=hm: kTm[:, hm, kt * 128:(kt + 1) * 128], RkTm,
                                   lambda kt, hm=hm: vm[:, kt, hm * 128:(hm + 1) * 128], Rvm, S,
                                   lambda a, b_, hh=hh: oTg[:, hh, a:b_], RoTg[hh], pTs, RpTs, rl, Rrl, moba=None)
                if hh == 3:
                    for t in range(NT):
                        for half in range(2):
                            i = pj[0] % 2
                            pj[0] += 1
                            ps, Rp = pf[i], Rpf[i]
                            for c in range(4):
                                MM(ps[:, :], oTg[:, c, t * 128:(t + 1) * 128], woutg[:, c, half * 512:(half + 1) * 512],
                                   c == 0, c == 3, [RoTg[c], Rwoutg], [Rp])
                            TT(x_sb[:, t, half * 512:(half + 1) * 512], x_sb[:, t, half * 512:(half + 1) * 512], ps[:, :],
                               ALU.add, [Rp, Rx[t]], [Rx[t]])
            em.barrier()

        ffn_state = {"n": 0}

        def ffn_phase(experts, gw=None, Rgw=None):
            PACE = PACE_N if gw is not None else 0
            cv = Carver()
            wgu = [cv.take([128, KC, 2, 512], BF16) for _ in range(2)]
            wdn = [cv.take([128, 4, D], BF16) for _ in range(2)]
            Rw = [R(), R()]
            act = cv.take([128, 4, S], BF16)
            Ract = [R() for _ in range(4)]
            sg = [cv.take([128, 512], F32) for _ in range(2)]
            Rsg = [R(), R()]
            units = [(e, fb) for e in experts for fb in range(NFB)]
            pace_buf = cv.take([128, max(PACE, 8)], F32)
            Rpace = R()

            def load(u):
                e, fb = units[u]
                sl = u % 2
                em.dma("pool", "wgu%d" % sl, wgu[sl].rearrange("p a b c -> p (a b c)"), wgu_d[e, fb], writes=[Rw[sl]])
                em.dma("pool", "wgu%d" % sl, wdn[sl].rearrange("p a b -> p (a b)"), wd_d[e, fb], writes=[Rw[sl]])

            load(0)
            ctr = 0
            for u, (e, fb) in enumerate(units):
                sl = u % 2
                if u + 1 < len(units):
                    load(u + 1)
                for tg in range(4):
                    for fc in range(4):
                        i = ctr % 2
                        ctr += 1
                        gp, Rgp = pf[i], Rpf[i]
                        up, Rup = pf[2 + i], Rpf[2 + i]
                        rds = [Rw[sl]] + RhT[tg * 4:tg * 4 + 4] + ([Rpace] if PACE else [])
                        for kc in range(KC):
                            MM(gp[:, :], wgu[sl][:, kc, 0, fc * 128:(fc + 1) * 128], hT[:, kc, tg * 512:(tg + 1) * 512],
                               kc == 0, kc == KC - 1, rds, [Rgp])
                        for kc in range(KC):
                            MM(up[:, :], wgu[sl][:, kc, 1, fc * 128:(fc + 1) * 128], hT[:, kc, tg * 512:(tg + 1) * 512],
                               kc == 0, kc == KC - 1, rds, [Rup])
                        if PACE:
                            MEMSET(pace_buf[:, 0:PACE], 0.0, [Rpace], reads=[Rgp, Rup])
                        ACT(sg[i][:], gp[:, :], AF.Silu, [Rgp], [Rsg[i]])
                        TT(act[:, fc, tg * 512:(tg + 1) * 512], sg[i][:], up[:, :], ALU.mult, [Rsg[i], Rup], [Ract[fc]])
                for t in range(NT):
                    for half in range(2):
                        i = ctr % 2
                        ctr += 1
                        yp, Ryp = pf[4 + i], Rpf[4 + i]
                        pace_here = PACE and (t * 2 + half) % 4 == 0
                        for fc in range(4):
                            MM(yp[:, :], act[:, fc, t * 128:(t + 1) * 128], wdn[sl][:, fc, half * 512:(half + 1) * 512],
                               fc == 0, fc == 3, [Ract[fc], Rw[sl]] + ([Rpace] if pace_here else []), [Ryp])
                        if PACE and (t * 2 + half) % 4 == 3:
                            MEMSET(pace_buf[:, 0:PACE], 0.0, [Rpace], reads=[Ryp])
                        xs_ = x_sb[:, t, half * 512:(half + 1) * 512]
                        if gw is None:
                            TT(xs_, xs_, yp[:, :], ALU.add, [Ryp, Rx[t]], [Rx[t]])
                        else:
                            STT(xs_, yp[:, :], gw[:, t, e - 1:e], xs_, ALU.mult, ALU.add, [Ryp, Rx[t], Rgw], [Rx[t]])
            em.barrier()

        def mixer_ssd(s):
            cv = Carver(ARENA_FULL)
            kTm = cv.take([128, 4, 256], BF16)
            RkTm = R()
            vm = cv.take([128, 2, 512], BF16)
            Rvm = R()
            mark = cv.off
            mem_kv(s, 1, cv, kTm, RkTm, vm, Rvm)
            em.barrier()
            cv.off = mark
            wblk = [cv.take([128, 4096], BF16) for _ in range(3)]
            Rwblk = [R() for _ in range(3)]
            hTc = [cv.take([128, KC, 128], BF16) for _ in range(2)]
            RhTc = [R(), R()]
            sz = cv.take([128, 1536], F32)
            Rsz = R()
            pre = [cv.take([128, 4, 131], F32) for _ in range(2)]
            Rpre = [R(), R()]
            halo = cv.take([128, 20, 3], F32)
            Rhalo = [R() for _ in range(5)]
            cacc = [cv.take([128, 128], F32) for _ in range(2)]
            Rcacc = [R(), R()]
            csf = [cv.take([128, 128], F32) for _ in range(2)]
            Rcsf = [R(), R()]
            xs_tok = cv.take([128, 1536], F32)
            Rxs = R()
            BT = cv.take([128, 4, 128], BF16)
            RBT = R()
            CT = cv.take([128, 4, 128], BF16)
            RCT = R()
            Btok = cv.take([128, 512], BF16)
            RBtok = R()
            dtv = cv.take([128, 24], F32)
            la = cv.take([128, 24], F32)
            lacs = cv.take([128, 24], F32)
            tot = cv.take([128, 24], F32)
            expcs = cv.take([128, 24], F32)
            dte = cv.take([128, 24], F32)
            cdec = cv.take([128, 24], F32)
            Rdt = R()
            xdt = cv.take([128, 1536], BF16)
            Rxdt = R()
            xdte = cv.take([128, 1536], BF16)
            Rxdte = R()
            lh = [cv.take([128, 128], F32) for _ in range(4)]
            Rlh = [R() for _ in range(4)]
            dec = [cv.take([128, 512], F32) for _ in range(2)]
            Rdec = [R(), R()]
            scT = [cv.take([128, 4, 128], BF16) for _ in range(2)]
            RscT = [R(), R()]
            CBm = [cv.take([128, 128], F32) for _ in range(2)]
            RCBm = [R(), R()]
            hst = cv.take([128, 4, 384], F32)
            hbf = cv.take([128, 4, 384], BF16)
            Rhst = [R() for _ in range(4)]
            Rhbf = [R() for _ in range(4)]
            y_sb = cv.take([128, 1536], F32)
            Ry = R()
            ytmp = cv.take([128, 1536], F32)
            Rytmp = R()
            yoff = cv.take([128, 384], F32)
            Ryoff = R()
            gss = cv.take([128, 4], F32)
            Rgss = R()
            un = cv.take([128, 1536], BF16)
            Run = R()
            ycT = cv.take([128, 16, 128], BF16)
            RycT = [R() for _ in range(16)]
            qmT = cv.take([128, 4, 128], BF16)
            RqmT = R()
            pTs = [cv.take([128, 512], BF16) for _ in range(3)]
            RpTs = [R() for _ in range(3)]
            rl = cv.take([128, 512], F32)
            Rrl = R()
            gssd = cv.take([128, 1536], F32)
            Rgssd = R()
            em.dma("sp", "gssd", gssd, gssd_d, writes=[Rgssd])
            MEMSET(halo.rearrange("p a b -> p (a b)"), 0.0, Rhalo)
            MEMSET(hst.rearrange("p a b -> p (a b)"), 0.0, Rhst)
            MEMSET(hbf.rearrange("p a b -> p (a b)"), 0.0, Rhbf)

            NU = 13

            def load_unit(u):
                c, j = divmod(u, NU)
                if c >= NT:
                    return
                sl = u % 3
                em.dma("sp", "wblk%d" % sl, wblk[sl], wl1_bf[j], reads=[Rwl1[j]], writes=[Rwblk[sl]])

            load_unit(0)
            load_unit(1)
            pj = [0]
            tc = [0]
            norm_tile(x_sb[:, 0, :], Rx[0], 1, hTc[0], RhTc[0], ss[:, 0:1], rstd[:, 0:1], Rss[0], Rrstd[0])
            for c in range(NT):
                ci = c % 2
                hc = hTc[ci]
                Rhc = RhTc[ci]
                P2, RP2 = pf[2], Rpf[2]
                for kc in range(KC):
                    MM(P2[:, 0:24], hc[:, kc, :], wdt_sb[:, kc * 24:(kc + 1) * 24], kc == 0, kc == KC - 1, [Rhc, Rwdt], [RP2])
                TT(dtv[:], P2[:, 0:24], dtb[:], ALU.add, [RP2, Rdtb], [Rdt])
                ACT(dtv[:], dtv[:], AF.Exp, [Rdt], [Rdt])
                ACT(dtv[:], dtv[:], AF.Ln, [Rdt, Ronec], [Rdt], bias=onec[:, 0:1])
                TT(la[:], dtv[:], a_bc[:], ALU.mult, [Rdt, Ra_bc], [Rdt])
                for j in range(9):
                    u = c * NU + j
                    load_unit(u + 2)
                    sl = u % 3
                    w3 = wblk[sl].rearrange("p (a b) -> p a b", b=512)
                    i = pj[0] % 2
                    pj[0] += 1
                    ps, Rp = pf[i], Rpf[i]
                    if j < 3:
                        for kc in range(KC):
                            MM(ps[:, :], hc[:, kc, :], w3[:, kc, :], kc == 0, kc == KC - 1, [Rhc, Rwblk[sl]], [Rp])
                        ACT(sz[:, j * 512:(j + 1) * 512], ps[:, :], AF.Silu, [Rp], [Rsz])
                    elif j < 8:
                        jb = j - 3
                        for q in range(4):
                            for kc in range(KC):
                                MM(ps[:, q * 128:(q + 1) * 128], w3[:, kc, q * 128:(q + 1) * 128], hc[:, kc, :],
                                   kc == 0, kc == KC - 1, [Rhc, Rwblk[sl]], [Rp], inc=(kc == KC - 1 and q == 3))
                        pi = jb % 2
                        CP(pre[pi][:, :, 0:3], halo[:, jb * 4:(jb + 1) * 4, :], [Rhalo[jb]], [Rpre[pi]], eng="pool")
                        ACT(pre[pi][:, :, 3:131], ps[:].rearrange("p (a b) -> p a b", b=128), AF.Copy, [Rp], [Rpre[pi]])
                        CP(halo[:, jb * 4:(jb + 1) * 4, :], pre[pi][:, :, 128:131], [Rpre[pi]], [Rhalo[jb]], eng="pool")
                        for q in range(4):
                            cc = jb * 4 + q
                            k2 = tc[0] % 2
                            tc[0] += 1
                            ceng = "dve"
                            TS(cacc[k2][:], pre[pi][:, q, 0:128], convw[:, cc * 4:cc * 4 + 1], None, ALU.mult,
                               reads=[Rpre[pi], Rconvw], writes=[Rcacc[k2]], eng=ceng)
                            for k in range(1, 4):
                                STT(cacc[k2][:], pre[pi][:, q, k:k + 128], convw[:, cc * 4 + k:cc * 4 + k + 1], cacc[k2][:],
                                    ALU.mult, ALU.add, [Rpre[pi], Rconvw, Rcacc[k2]], [Rcacc[k2]], eng=ceng)
                            if cc < 12:
                                ACT(csf[k2][:], cacc[k2][:], AF.Silu, [Rcacc[k2], Rconvb], [Rcsf[k2]], bias=convb[:, cc:cc + 1])
                                P3, RP3 = pf[3], Rpf[3]
                                TR(P3[:, q * 128:(q + 1) * 128], csf[k2][:], identf[:], [Rcsf[k2], Ridentf], [RP3])
                                if q == 3:
                                    CP(xs_tok[:, jb * 512:(jb + 1) * 512], P3[:, :], [RP3], [Rxs])
                            elif cc < 16:
                                g = cc - 12
                                ACT(BT[:, g, :], cacc[k2][:], AF.Silu, [Rcacc[k2], Rconvb], [RBT], bias=convb[:, cc:cc + 1])
                                TR(pb[0][:, g * 128:(g + 1) * 128], BT[:, g, :], identb[:], [RBT, Rident], [Rpb[0]])
                                if g == 3:
                                    CP(Btok[:], pb[0][:, 0:512], [Rpb[0]], [RBtok])
                            else:
                                g = cc - 16
                                ACT(CT[:, g, :], cacc[k2][:], AF.Silu, [Rcacc[k2], Rconvb], [RCT], bias=convb[:, cc:cc + 1])
                    else:
                        for hm in range(4):
                            for kc in range(KC):
                                MM(ps[:, hm * 128:(hm + 1) * 128], w3[:, kc, hm * 128:(hm + 1) * 128], hc[:, kc, :],
                                   kc == 0, kc == KC - 1, [Rhc, Rwblk[sl]], [Rp], inc=(kc == KC - 1 and hm == 3))
                        ACT(qmT[:], ps[:].rearrange("p (a b) -> p a b", b=128), AF.Copy, [Rp], [RqmT], scale=SCALE)
                MM(P2[:, 0:24], U_sb[:], la[:], True, True, [RU, Rdt], [RP2], inc=False)
                MM(P2[:, 24:48], onesf[:], la[:], True, True, [Ronesf, Rdt], [RP2], inc=True)
                CP(lacs[:], P2[:, 0:24], [RP2], [Rdt])
                CP(tot[:], P2[:, 24:48], [RP2], [Rdt])
                ACT(expcs[:], lacs[:], AF.Exp, [Rdt], [Rdt])
                TT(dte[:], tot[:], lacs[:], ALU.subtract, [Rdt], [Rdt])
                ACT(dte[:], dte[:], AF.Exp, [Rdt], [Rdt])
                ACT(cdec[:], tot[:], AF.Exp, [Rdt], [Rdt])
                if c + 1 < NT:
                    cn = c + 1
                    norm_tile(x_sb[:, cn, :], Rx[cn], 1, hTc[cn % 2], RhTc[cn % 2], ss[:, cn:cn + 1], rstd[:, cn:cn + 1],
                              Rss[cn], Rrstd[cn])
                for hm in range(4):
                    attention_head(qmT[:, hm, :], RqmT,
                                   lambda kt, hm=hm: kTm[:, hm, kt * 128:(kt + 1) * 128], RkTm,
                                   lambda kt, hm=hm: vm[:, kt, hm * 128:(hm + 1) * 128], Rvm, 128,
                                   lambda a, b_, hm=hm: ycT[:, 12 + hm, a:b_], RycT[12 + hm], pTs, RpTs, rl, Rrl, moba=None)
                v3 = lambda a: a.rearrange("p (a b) -> p a b", b=64)
                b3 = lambda a, n: a.unsqueeze(2).to_broadcast([128, n, 64])
                TT(v3(xdt[:]), v3(xs_tok[:]), b3(dtv[:], 24), ALU.mult, [Rxs, Rdt], [Rxdt])
                TT(dte[:], dte[:], dtv[:], ALU.mult, [Rdt], [Rdt])
                TT(v3(xdte[:]), v3(xs_tok[:]), b3(dte[:], 24), ALU.mult, [Rxs, Rdt], [Rxdte])
                for g in range(4):
                    gi = g % 2
                    MM(P2[:, 128:256], BT[:, g, :], CT[:, g, :], True, True, [RBT, RCT], [RP2])
                    TT(CBm[gi][:], P2[:, 128:256], U_sb[:], ALU.mult, [RP2, RU], [RCBm[gi]])
                    for part, (h0, nh) in enumerate(((0, 4), (4, 2))):
                        bi = part
                        SEG, RSEG = pf[3 + bi], Rpf[3 + bi]
                        for hh in range(nh):
                            h = 6 * g + h0 + hh
                            li = (h0 + hh) % 4
                            TS(lh[li][:], SL_sb[:], la[:, h:h + 1], None, ALU.mult, reads=[RSL, Rdt], writes=[Rlh[li]], eng="pool")
                            MM(SEG[:, hh * 128:(hh + 1) * 128], lh[li][:], U_sb[:], True, True, [Rlh[li], RU], [RSEG],
                               inc=(hh == nh - 1))
                        ACT(dec[bi][:, 0:nh * 128], SEG[:, 0:nh * 128], AF.Exp, [RSEG], [Rdec[bi]])
                        TT(scT[bi][:, 0:nh, :], dec[bi][:, 0:nh * 128].rearrange("p (a b) -> p a b", b=128),
                           CBm[gi][:].unsqueeze(1).to_broadcast([128, nh, 128]), ALU.mult, [Rdec[bi], RCBm[gi]], [RscT[bi]])
                        for hh in range(nh):
                            h = 6 * g + h0 + hh
                            hl = h0 + hh
                            MM(pf[5][:, hl * 64:(hl + 1) * 64], scT[bi][:, hh, :], xdt[:, h * 64:(h + 1) * 64], True, True,
                               [RscT[bi], Rxdt], [Rpf[5]], inc=(hl == 5))
                    MM(pf[0][:, 0:384], CT[:, g, :], hbf[:, g, :], True, True, [RCT, Rhbf[g]], [Rpf[0]])
                    MM(pf[1][:, 0:384], Btok[:, g * 128:(g + 1) * 128], xdte[:, g * 384:(g + 1) * 384], True, True,
                       [RBtok, Rxdte], [Rpf[1]])
                    TT(v3(yoff[:]), v3(pf[0][:, 0:384]), b3(expcs[:, 6 * g:6 * g + 6], 6), ALU.mult, [Rpf[0], Rdt], [Ryoff])
                    TT(y_sb[:, g * 384:(g + 1) * 384], pf[5][:, 0:384], yoff[:], ALU.add, [Rpf[5], Ryoff], [Ry])
                    TT(v3(hst[:, g, :]), v3(hst[:, g, :]), b3(cdec[:, 6 * g:6 * g + 6], 6), ALU.mult, [Rhst[g], Rdt], [Rhst[g]])
                    TT(hst[:, g, :], hst[:, g, :], pf[1][:, 0:384], ALU.add, [Rhst[g], Rpf[1]], [Rhst[g]])
                    CP(hbf[:, g, :], hst[:, g, :], [Rhst[g]], [Rhbf[g]], eng="pool")
                TT(v3(ytmp[:]), v3(xs_tok[:]), b3(dskip[:], 24), ALU.mult, [Rxs, Rdskip], [Rytmp])
                TT(y_sb[:], y_sb[:], ytmp[:], ALU.add, [Ry, Rytmp], [Ry])
                TT(y_sb[:], y_sb[:], sz[:], ALU.mult, [Ry, Rsz], [Ry])
                MEMSET(gss[:], 0.0, [Rgss])
                for g in range(4):
                    ACT(junk[:, 0:384], y_sb[:, g * 384:(g + 1) * 384], AF.Square, [Ry, Rgss], [Rgss], accum_out=gss[:, g:g + 1])
                ACT(gss[:], gss[:], AF.Sqrt, [Rgss, Reps], [Rgss], bias=epsc[:, 0:1], scale=1.0 / 384)
                RECIP(gss[:], gss[:], [Rgss], [Rgss])
                g4 = lambda a: a.rearrange("p (a b) -> p a b", b=384)
                TT(g4(ytmp[:]), g4(y_sb[:]), gss[:].unsqueeze(2).to_broadcast([128, 4, 384]), ALU.mult, [Ry, Rgss], [Rytmp])
                TT(un[:], ytmp[:], gssd[:], ALU.mult, [Rytmp, Rgssd], [Run])
                for cc in range(12):
                    bi = 0 if cc < 8 else 1
                    off = cc if cc < 8 else cc - 8
                    TR(pb[bi][:, off * 128:(off + 1) * 128], un[:, cc * 128:(cc + 1) * 128], identb[:], [Run, Rident], [Rpb[bi]],
                       inc=(cc in (7, 11)))
                CP(ycT[:, 0:8, :], pb[0][:].rearrange("p (a b) -> p a b", b=128), [Rpb[0]], RycT[0:8])
                ACT(ycT[:, 8:12, :], pb[1][:, 0:512].rearrange("p (a b) -> p a b", b=128), AF.Copy, [Rpb[1]], RycT[8:12])
                for jb in range(4):
                    u = c * NU + 9 + jb
                    load_unit(u + 2)
                    sl = u % 3
                    w3 = wblk[sl].rearrange("p (a b) -> p a b", b=1024)
                    for q in range(4):
                        cidx = jb * 4 + q
                        for half in range(2):
                            MM(pf[half][:, :], ycT[:, cidx, :], w3[:, q, half * 512:(half + 1) * 512],
                               cidx == 0, cidx == 15, [RycT[cidx], Rwblk[sl]], [Rpf[half]],
                               inc=(q == 3 and half == 1))
                for half in range(2):
                    xs_ = x_sb[:, c, half * 512:(half + 1) * 512]
                    TT(xs_, xs_, pf[half][:, :], ALU.add, [Rpf[half], Rx[c]], [Rx[c]])
            em.barrier()

        def router():
            cv = Carver()
            lg = cv.take([128, 128], F32)
            lg2 = cv.take([128, 128], F32)
            eq = cv.take([128, 128], F32)
            mx1 = cv.take([128, 16], F32)
            mx2 = cv.take([128, 16], F32)
            Rl = R()
            G, RG = pf[0], Rpf[0]
            for t in range(NT):
                for kc in range(KC):
                    MM(G[:, t * 8:(t + 1) * 8], hT[:, kc, t * 128:(t + 1) * 128], wr_sb[:, kc * 8:(kc + 1) * 8],
                       kc == 0, kc == KC - 1, [RhT[t], Rwr], [RG], inc=(kc == KC - 1 and t == NT - 1))
            g3 = lambda a: a.rearrange("p (a b) -> p a b", b=8)
            bc = lambda a: a.unsqueeze(2).to_broadcast([128, 16, 8])
            TT(g3(lg[:]), g3(G[:, 0:128]), br_sb[:].unsqueeze(1).to_broadcast([128, 16, 8]), ALU.add, [RG, Rbr], [Rl])
            RED(mx1[:], g3(lg[:]), ALU.max, [Rl], [Rl])
            TT(g3(eq[:]), g3(lg[:]), bc(mx1[:]), ALU.is_equal, [Rl], [Rl])
            STT(lg2[:], eq[:], -BIG, lg[:], ALU.mult, ALU.add, [Rl], [Rl])
            RED(mx2[:], g3(lg2[:]), ALU.max, [Rl], [Rl])
            TT(g3(eq[:]), g3(lg[:]), bc(mx2[:]), ALU.is_ge, [Rl], [Rl])
            TT(g3(lg2[:]), g3(lg[:]), bc(mx1[:]), ALU.subtract, [Rl], [Rl])
            ACT(lg2[:], lg2[:], AF.Exp, [Rl], [Rl])
            TT(lg2[:], lg2[:], eq[:], ALU.mult, [Rl], [Rl])
            RED(mx1[:], g3(lg2[:]), ALU.add, [Rl], [Rl])
            RECIP(mx1[:], mx1[:], [Rl], [Rl])
            TT(g3(gw[:]), g3(lg2[:]), bc(mx1[:]), ALU.mult, [Rl], [Rgw])
            em.barrier()

        def layer1(s):
            mixer_ssd(s)
            if DBG == "nomoe":
                return
            norm_seq(3)
            router()
            nexp = 9 if not (DBG or "").startswith("moe") else 1 + int(DBG[3:])
            ffn_phase(list(range(1, nexp)), gw=gw[:].rearrange("p (a b) -> p a b", b=8), Rgw=Rgw)

        def final_norm(s):
            cv = Carver()
            ot = [cv.take([128, D], F32) for _ in range(2)]
            Rot = [R(), R()]
            for t in range(NT):
                i = t % 2
                MEMSET(ss[:, t:t + 1], 0.0, [Rss[t]])
                ACT(junk[:], x_sb[:, t, :], AF.Square, [Rx[t], Rss[t]], [Rss[t]], accum_out=ss[:, t:t + 1])
                ACT(rstd[:, t:t + 1], ss[:, t:t + 1], AF.Sqrt, [Rss[t], Reps], [Rrstd[t]], bias=epsc[:, 0:1], scale=1.0 / D)
                RECIP(rstd[:, t:t + 1], rstd[:, t:t + 1], [Rrstd[t]], [Rrstd[t]])
                STT(ot[i][:], x_sb[:, t, :], rstd[:, t:t + 1], gfin[:], ALU.mult, ALU.mult,
                    [Rx[t], Rrstd[t], Rgfin], [Rot[i]])
                em.dma("sp", "out", out_d[s, t * 128:(t + 1) * 128, :], ot[i][:], reads=[Rot[i]], writes=[Rout])
            em.barrier()

        Rout = R()

        wl1_bf = nc.dram_tensor("wl1_bf", [13, 128, 4096], BF16, kind="Internal").ap()
        Rwl1 = [R() for _ in range(13)]
        for j in range(13):
            src = wssd_d[j] if j < 9 else wout_d[1][:, (j - 9) * 4096:(j - 8) * 4096]
            em.dma("pool", "wl1cast%d" % (j % 4), wl1_bf[j], src, writes=[Rwl1[j]])

        for s in range(n_seq):
            for t4 in range(4):
                em.dma("sp", "xload%d" % t4, x_sb[:, t4 * 4:(t4 + 1) * 4, :],
                       x_d[s, t4 * 512:(t4 + 1) * 512, :].rearrange("(t p) d -> p t d", p=128),
                       writes=Rx[t4 * 4:(t4 + 1) * 4])
            norm_seq(0)
            mixer_moba(s)
            norm_seq(2)
            ffn_phase([0])
            if n_layers > 1:
                layer1(s)
            final_norm(s)

        em.finish()
    return nc


_SHARED_CACHE = {}


def _run(inputs, n_layers=2, n_seq=2, n_cores=8, core0=0):
    inp = {k: np.asarray(v) for k, v in inputs.items()}
    sh = _prep_shared(inp)
    nc = build(n_layers=n_layers, n_seq=n_seq)
    in_maps = []
    for c in range(core0, core0 + n_cores):
        m = dict(sh)
        m["x"] = np.ascontiguousarray(inp["x"][n_seq * c:n_seq * (c + 1)], dtype=np.float32)
        m["mem"] = np.ascontiguousarray(inp["mem"][n_seq * c:n_seq * (c + 1)], dtype=np.float32)
        in_maps.append(m)
    res = run_bass_kernel_spmd(nc, in_maps, core_ids=list(range(n_cores)))
    return np.concatenate([np.asarray(r["out"]) for r in res.results], axis=0)


N_CORES = 8
SEQ_PER_CORE = 16 // N_CORES


def kernel(**inputs):
    return _run(inputs, n_layers=2, n_seq=SEQ_PER_CORE, n_cores=N_CORES).astype(np.float32)
```
